# Optimizing a Trainium2 kernel written in Bass

```python
import jax, jax.numpy as jnp
from jax import lax
import numpy as np

D_MODEL = 2048
BATCH = 4
SEQ = 4096
DEPTH = 2

D_MIX = D_MODEL
GLA_HEADS = 4
GLA_DV = D_MIX // 2 // GLA_HEADS
GLA_DK = GLA_DV // 2
GLA_GATE_RANK = 16
GLA_TAU = 16.0
GLA_CHUNK = 64
RET_HEADS = 4
RET_DV = D_MIX // 2 // RET_HEADS
RET_DK = RET_DV
RET_CHUNK = 64
ROPE_BASE = 10000.0
D_FF = 4 * D_MODEL
EPS = 1e-6
MAX_POS_OFFSET = 1024

GLA_QK = GLA_HEADS * GLA_DK
GLA_V = GLA_HEADS * GLA_DV
RET_QK = RET_HEADS * RET_DK
RET_V = RET_HEADS * RET_DV
D_IN_PROJ = 2 * GLA_QK + 2 * GLA_V + GLA_GATE_RANK + 2 * RET_QK + 2 * RET_V

kernel_name = "hybrid_gla_retention_sqrelu"


def _split_points():
    widths = [GLA_QK, GLA_QK, GLA_V, GLA_V, GLA_GATE_RANK, RET_QK, RET_QK, RET_V, RET_V]
    pts, acc = [], 0
    for w in widths[:-1]:
        acc += w
        pts.append(acc)
    return pts


def rms_norm(x, g):
    xf = x.astype(jnp.float32)
    y = xf * lax.rsqrt(jnp.mean(xf * xf, axis=-1, keepdims=True) + EPS)
    return (y * g.astype(jnp.float32)).astype(x.dtype)


def head_rms_norm(o, n_heads, g):
    b, s, _ = o.shape
    of = o.astype(jnp.float32).reshape(b, s, n_heads, -1)
    y = of * lax.rsqrt(jnp.mean(of * of, axis=-1, keepdims=True) + EPS)
    return y.reshape(b, s, -1) * g.astype(jnp.float32)


def head_group_norm(o, n_heads, g, beta):
    b, s, _ = o.shape
    of = o.astype(jnp.float32).reshape(b, s, n_heads, -1)
    mu = jnp.mean(of, axis=-1, keepdims=True)
    c = of - mu
    y = c * lax.rsqrt(jnp.mean(c * c, axis=-1, keepdims=True) + EPS)
    return y.reshape(b, s, -1) * g.astype(jnp.float32) + beta.astype(jnp.float32)


def to_chunks(t, n_heads, chunk):
    b, s, _ = t.shape
    return t.reshape(b, s // chunk, chunk, n_heads, -1).transpose(0, 3, 1, 2, 4)


def from_chunks(t):
    b, h, n, c, d = t.shape
    return t.transpose(0, 2, 3, 1, 4).reshape(b, n * c, h * d)


def chunk_state_scan(decay, local):
    def step(s, inp):
        d, u = inp
        return d[..., None] * s + u, s
    s0 = jnp.zeros_like(local[:, :, 0])
    _, states = lax.scan(step, s0, (jnp.moveaxis(decay, 2, 0), jnp.moveaxis(local, 2, 0)))
    return jnp.moveaxis(states, 0, 2)


def gla_mixer(q, k, v, log_a):
    c = GLA_CHUNK
    q = to_chunks(q, GLA_HEADS, c) * (GLA_DK ** -0.5)
    k = to_chunks(k, GLA_HEADS, c)
    v = to_chunks(v, GLA_HEADS, c)
    g = to_chunks(log_a.astype(jnp.float32), GLA_HEADS, c)
    b = jnp.cumsum(g, axis=3)
    b_last = b[:, :, :, -1:, :]
    q_dec = q * jnp.exp(b)
    k_dec = k * jnp.exp(-b)
    k_tail = k * jnp.exp(b_last - b)
    causal = jnp.tril(jnp.ones((c, c), dtype=bool))
    scores = jnp.where(causal, jnp.einsum('bhnik,bhnjk->bhnij', q_dec, k_dec), 0.0)
    o_intra = jnp.einsum('bhnij,bhnjv->bhniv', scores, v)
    local = jnp.einsum('bhnjk,bhnjv->bhnkv', k_tail, v)
    states = chunk_state_scan(jnp.exp(b_last[:, :, :, 0, :]), local)
    o_inter = jnp.einsum('bhnik,bhnkv->bhniv', q_dec, states)
    return from_chunks(o_intra + o_inter)


def apply_rotary(t, cos, sin):
    half = t.shape[-1] // 2
    t1, t2 = t[..., :half], t[..., half:]
    return jnp.concatenate([t1 * cos - t2 * sin, t2 * cos + t1 * sin], axis=-1)


def retention_mixer(q, k, v, cos, sin):
    c = RET_CHUNK
    bsz, s, _ = q.shape
    q = apply_rotary(q.reshape(bsz, s, RET_HEADS, RET_DK), cos, sin).reshape(bsz, s, -1)
    k = apply_rotary(k.reshape(bsz, s, RET_HEADS, RET_DK), cos, sin).reshape(bsz, s, -1)
    q = to_chunks(q, RET_HEADS, c)
    k = to_chunks(k, RET_HEADS, c) * (RET_DK ** -0.5)
    v = to_chunks(v, RET_HEADS, c)
    n_chunks = q.shape[2]
    heads = jnp.arange(RET_HEADS, dtype=jnp.float32)
    log_gamma = jnp.log1p(-(2.0 ** (-5.0 - heads)))
    idx = jnp.arange(c, dtype=jnp.float32)
    rel = idx[:, None] - idx[None, :]
    decay_mask = jnp.where(rel >= 0.0,
                           jnp.exp(log_gamma[:, None, None] * jnp.maximum(rel, 0.0)), 0.0)
    scores = jnp.einsum('bhnid,bhnjd->bhnij', q, k) * decay_mask[None, :, None]
    o_intra = jnp.einsum('bhnij,bhnjv->bhniv', scores, v)
    inner = jnp.exp(log_gamma[:, None] * (idx[None, :] + 1.0))
    tail = jnp.exp(log_gamma[:, None] * (c - 1.0 - idx[None, :]))
    local = jnp.einsum('bhnjd,bhnjv->bhndv', k * tail[None, :, None, :, None], v)
    chunk_decay = jnp.broadcast_to(jnp.exp(log_gamma * c)[None, :, None, None],
                                   (bsz, RET_HEADS, n_chunks, RET_DK))
    states = chunk_state_scan(chunk_decay, local)
    o_inter = jnp.einsum('bhnid,bhndv->bhniv', q, states) * inner[None, :, None, :, None]
    return from_chunks(o_intra + o_inter)


def setup_inputs(seed: int = 0) -> dict:
    key = jax.random.key(seed)
    ks = jax.random.split(key, 16)
    f32 = jnp.float32
    x = jax.random.normal(ks[0], (BATCH, SEQ, D_MODEL), f32)
    offsets = jax.random.randint(ks[1], (BATCH, 1), 0, MAX_POS_OFFSET, dtype=jnp.int32)
    positions = offsets + jnp.arange(SEQ, dtype=jnp.int32)[None, :]
    def gain(k, n):
        return 1.0 + 0.02 * jax.random.normal(k, (DEPTH, n), f32)
    return {
        "x": x,
        "positions": positions,
        "attn_norm": gain(ks[2], D_MODEL),
        "w_in": jax.random.normal(ks[3], (DEPTH, D_MODEL, D_IN_PROJ), f32) * D_MODEL ** -0.5,
        "gla_gate_up": jax.random.normal(ks[4], (DEPTH, GLA_GATE_RANK, GLA_QK), f32) * GLA_GATE_RANK ** -0.5,
        "gla_gate_bias": 0.1 * jax.random.normal(ks[5], (DEPTH, GLA_QK), f32),
        "gla_out_norm": gain(ks[6], GLA_V),
        "ret_norm_gain": gain(ks[7], RET_V),
        "ret_norm_bias": 0.02 * jax.random.normal(ks[8], (DEPTH, RET_V), f32),
        "w_out": jax.random.normal(ks[9], (DEPTH, D_MIX, D_MODEL), f32) * D_MIX ** -0.5,
        "mlp_norm": gain(ks[10], D_MODEL),
        "w_up": jax.random.normal(ks[11], (DEPTH, D_MODEL, D_FF), f32) * D_MODEL ** -0.5,
        "w_down": jax.random.normal(ks[12], (DEPTH, D_FF, D_MODEL), f32) * D_FF ** -0.5,
        "final_norm": 1.0 + 0.02 * jax.random.normal(ks[13], (D_MODEL,), f32),
    }


def reference(x, positions, attn_norm, w_in, gla_gate_up, gla_gate_bias, gla_out_norm,
              ret_norm_gain, ret_norm_bias, w_out, mlp_norm, w_up, w_down, final_norm):
    inv_freq = ROPE_BASE ** (-jnp.arange(0, RET_DK, 2, dtype=jnp.float32) / RET_DK)
    ang = positions.astype(jnp.float32)[..., None] * inv_freq
    cos = jnp.cos(ang)[:, :, None, :]
    sin = jnp.sin(ang)[:, :, None, :]
    split_pts = _split_points()
    for l in range(DEPTH):
        h = rms_norm(x, attn_norm[l])
        proj = h @ w_in[l]
        gq, gk, gv, gg, gr, rq, rk, rv, rg = jnp.split(proj, split_pts, axis=-1)
        log_a = jax.nn.log_sigmoid((gr @ gla_gate_up[l] + gla_gate_bias[l]).astype(jnp.float32)) / GLA_TAU
        o_gla = gla_mixer(gq, gk, gv, log_a)
        o_gla = head_rms_norm(o_gla, GLA_HEADS, gla_out_norm[l]) * jax.nn.silu(gg.astype(jnp.float32))
        o_ret = retention_mixer(rq, rk, rv, cos, sin)
        o_ret = head_group_norm(o_ret, RET_HEADS, ret_norm_gain[l], ret_norm_bias[l]) * jax.nn.silu(rg.astype(jnp.float32))
        mix = jnp.concatenate([o_gla, o_ret], axis=-1).astype(x.dtype)
        x = x + mix @ w_out[l]
        h = rms_norm(x, mlp_norm[l])
        x = x + jnp.square(jax.nn.relu(h @ w_up[l])) @ w_down[l]
    return rms_norm(x, final_norm)
```

```python
import contextlib
import numpy as np
import concourse.bass as bass
import concourse.mybir as mybir
from concourse.bass_utils import run_bass_kernel_spmd

F32 = mybir.dt.float32
BF16 = mybir.dt.bfloat16
I32 = mybir.dt.int32
AF = mybir.ActivationFunctionType
ALU = mybir.AluOpType

EPS = 1e-6
ROPE_BASE = 10000.0
TWO_PI = 2.0 * np.pi
CW1 = float(np.float32(6.28125))
CW2 = float(np.float32(TWO_PI - 6.28125))
RND = 12582912.0


class Cfg:
    def __init__(self, D=2048, T=2048, HG=4, HR=4, DFF=8192, L=2, TBF=1024):
        self.D, self.T, self.HG, self.HR, self.DFF, self.L, self.TBF = D, T, HG, HR, DFF, L, TBF
        self.KC = D // 128
        self.NT = T // 128
        self.QKG, self.VG = HG * 128, HG * 256
        self.QKR, self.VR = HR * 256, HR * 256
        self.DMIX = self.VG + self.VR
        self.KM = self.DMIX // 128
        self.o_gq = 0
        self.o_gk = self.QKG
        self.o_gv = 2 * self.QKG
        self.o_gg = self.o_gv + self.VG
        self.o_gr = self.o_gg + self.VG
        self.o_rq = self.o_gr + 16
        self.o_rk = self.o_rq + self.QKR
        self.o_rv = self.o_rk + self.QKR
        self.o_rg = self.o_rv + self.VR
        self.DIN = self.o_rg + self.VR
        self.NB = 2 * HG + 4 * HR
        self.NKD = HG + 2 * HR
        self.NH = HG + HR


class Buf:
    __slots__ = ("w", "r", "excl")

    def __init__(self, excl=False):
        self.w = {}
        self.r = {}
        self.excl = excl


class Eng:
    def __init__(self, ctx, name, handle):
        self.h = handle
        self.key = "e_" + name
        self.sem = ctx.nc.alloc_semaphore("es_" + name)
        ctx.sems[self.key] = self.sem
        self.cnt = 0
        self.waited = {}


class DSem:
    def __init__(self, ctx, name):
        self.ctx = ctx
        self.name = name
        self.sub = {}

    def for_queue(self, q):
        d = self.sub.get(q.key)
        if d is None:
            d = _DSub(self.ctx, self.name + "_" + q.key)
            self.sub[q.key] = d
            self.ctx.dsubs.append(d)
        return d


class _DSub:
    def __init__(self, ctx, name):
        self.key = "d_" + name
        self.sem = ctx.nc.alloc_semaphore("ds_" + name)
        ctx.sems[self.key] = self.sem
        self.cnt = 0


def _merge(d, s):
    for k, v in s.items():
        if d.get(k, 0) < v:
            d[k] = v


class Ctx:
    def __init__(self, nc):
        self.nc = nc
        self.sems = {}
        self.pe = Eng(self, "pe", nc.tensor)
        self.act = Eng(self, "act", nc.scalar)
        self.dve = Eng(self, "dve", nc.vector)
        self.pool = Eng(self, "pool", nc.gpsimd)
        self.sp = Eng(self, "sp", nc.sync)
        self.engs = [self.pe, self.act, self.dve, self.pool, self.sp]
        self.dsems = []
        self.dsubs = []
        self.dnames = {}
        self.dcache = {}

    def dsem(self, name):
        i = self.dnames.get(name, 0)
        self.dnames[name] = i + 1
        key = "%s_%d" % (name, i)
        d = self.dcache.get(key)
        if d is None:
            d = self.dcache[key] = DSem(self, key)
            self.dsems.append(d)
        return d

    def new_layer(self):
        self.dnames = {}

    def _wait(self, eng, deps):
        for key, val in deps.items():
            if eng.waited.get(key, 0) >= val:
                continue
            eng.h.wait_ge(self.sems[key], val)
            eng.waited[key] = val

    def _deps(self, eng, reads, writes):
        if any(b.excl for b in reads):
            writes = list(writes) + [b for b in reads if b.excl]
            reads = [b for b in reads if not b.excl]
        d = {}
        for b in reads:
            _merge(d, b.w)
        for b in writes:
            w = dict(b.w)
            r = dict(b.r)
            _merge(d, w)
            _merge(d, r)
        if eng is self.pe:
            d.pop(eng.key, None)
        return d

    def _mark(self, key, cnt, reads, writes):
        if any(b.excl for b in reads):
            writes = list(writes) + [b for b in reads if b.excl]
            reads = [b for b in reads if not b.excl]
        for b in writes:
            b.w = {key: cnt}
            b.r = {}
        for b in reads:
            if not any(b is x for x in writes):
                if b.r.get(key, 0) < cnt:
                    b.r[key] = cnt

    def op(self, eng, fn, reads=(), writes=()):
        self._wait(eng, self._deps(eng, reads, writes))
        inst = fn(eng.h)
        eng.cnt += 1
        inst.then_inc(eng.sem, 1)
        self._mark(eng.key, eng.cnt, reads, writes)
        return inst

    def pe_group(self, items, reads=(), writes=()):
        eng = self.pe
        self._wait(eng, self._deps(eng, reads, writes))
        inst = None
        for it in items:
            if it[0] == "mm":
                inst = eng.h.matmul(it[1], it[2], it[3], start=it[4], stop=it[5])
            else:
                inst = eng.h.transpose(it[1], it[2], it[3])
        eng.cnt += 1
        inst.then_inc(eng.sem, 1)
        self._mark(eng.key, eng.cnt, reads, writes)

    def dma(self, q, out_ap, in_ap, ds, reads=(), writes=()):
        ds = ds.for_queue(q)
        deps = self._deps(q, reads, writes)
        deps.pop(ds.key, None)
        self._wait(q, deps)
        inst = q.h.dma_start(out=out_ap, in_=in_ap)
        ds.cnt += 16
        inst.then_inc(ds.sem, 16)
        self._mark(ds.key, ds.cnt, reads, writes)

    def barrier(self):
        allv = {e.key: e.cnt for e in self.engs if e.cnt}
        for d in self.dsubs:
            if d.cnt:
                allv[d.key] = d.cnt
        for e in self.engs:
            self._wait(e, {k: v for k, v in allv.items() if k != e.key})


class _Stop(Exception):
    pass


def build(cfg, use_cc=True, pair_groups=None, stop=None):
    c = cfg
    D, T, HG, HR, DFF, L = c.D, c.T, c.HG, c.HR, c.DFF, c.L
    KC, NT, KM, NB, NKD = c.KC, c.NT, c.KM, c.NB, c.NKD
    DMIX, VG, VR, QKG = c.DMIX, c.VG, c.VR, c.QKG
    nc = bass.Bass("TRN2", target_bir_lowering=False)
    K = Ctx(nc)
    pe, act, dve, pool, sp = K.pe, K.act, K.dve, K.pool, K.sp

    def din(name, shape, dt=F32):
        return nc.dram_tensor(name, list(shape), dt, kind="ExternalInput").ap()

    x_in = din("x", [T, D])
    pos_in = din("pos", [128, T], I32)
    w_in = din("w_in", [L, D, c.DIN])
    gup_in = din("gup", [L, 17, QKG])
    an_in = din("an", [L, 128, D])
    mn_in = din("mn", [L, 128, D])
    fn_in = din("fn", [128, D])
    gon_in = din("gon", [L, 128, VG])
    rng_in = din("rng", [L, 128, VR])
    rnb_in = din("rnb", [L, 128, VR])
    w_out = din("w_out", [L, DMIX, D])
    w_up = din("w_up", [L, D, DFF])
    w_down = din("w_down", [L, DFF, D])
    ident_in = din("ident", [128, 128])
    tri_in = din("tri", [128, 128])
    mask_in = din("mask", [128, 128])
    rdec_in = din("rdec", [128, HR * 2 * 128])
    misc_in = din("misc", [128, 4])
    out = nc.dram_tensor("out", [T, D], F32, kind="ExternalOutput").ap()

    xs = nc.dram_tensor("xs", [T, D], F32).ap()
    qk_s = nc.dram_tensor("qk_s", [128, NB, T], BF16).ap()
    kd_s = nc.dram_tensor("kd_s", [T, NKD * 128], BF16).ap()
    v_s = nc.dram_tensor("v_s", [T, DMIX], BF16).ap()
    sg_s = nc.dram_tensor("sg_s", [T, DMIX], F32).ap()
    cs_s = nc.dram_tensor("cs_s", [2, 128, T], F32).ap()
    cc_in_t = [nc.dram_tensor("cc_in%d" % l, [NKD * 128, 256], F32) for l in range(L)]
    cc_out_t = [nc.dram_tensor("cc_out%d" % l, [2 * NKD * 128, 256], F32) for l in range(L)]

    dbufs = {}

    def DB(*key):
        b = dbufs.get(key)
        if b is None:
            b = dbufs[key] = Buf()
        return b

    banks = [nc.alloc_psum_tensor("bank%d" % i, [128, 512], F32) for i in range(8)]
    bankb = [Buf(excl=True) for _ in range(8)]

    def bf(ap):
        return ap.bitcast(BF16)

    es0 = contextlib.ExitStack()

    sbn = [0]

    def sb(es, name, shape, dt):
        sbn[0] += 1
        return es.enter_context(nc.sbuf_tensor("%s_s%d" % (name, sbn[0]), list(shape), dt))

    ident_b = sb(es0, "ident_b", [128, 128], BF16)
    tri_f = sb(es0, "tri_f", [128, 128], F32)
    mask_f = sb(es0, "mask_f", [128, 128], F32)
    misc = sb(es0, "misc", [128, 4], F32)
    d_all = sb(es0, "d_all", [128, HG * NT], F32)
    stat = sb(es0, "stat", [128, 64], F32)
    cB = Buf()
    dB, S1B, SbB = Buf(), [Buf() for _ in range(NKD)], [Buf() for _ in range(NKD)]
    cs = K.dsem("const")
    K.dma(pool, ident_b[:], ident_in, cs, writes=[cB])
    K.dma(sp, tri_f[:], tri_in, cs, writes=[cB])
    K.dma(sp, mask_f[:], mask_in, cs, writes=[cB])
    K.dma(sp, misc[:], misc_in, cs, writes=[cB])

    gammas = {0.0: 1.0}
    hgam = [1.0 - 2.0 ** (-5.0 - h) for h in range(HR)]

    with contextlib.ExitStack() as es:
        cosT = sb(es, "cosT", [128, T], F32)
        sinT = sb(es, "sinT", [128, T], F32)
        posi = sb(es, "posi", [128, T], I32)
        ang = sb(es, "ang", [128, T], F32)
        t1 = sb(es, "rt1", [128, T], F32)
        t2 = sb(es, "rt2", [128, T], F32)
        pB, aB, t1B, t2B, cosB, sinB = Buf(), Buf(), Buf(), Buf(), Buf(), Buf()
        ds = K.dsem("pos")
        K.dma(sp, posi[:], pos_in, ds, writes=[pB])
        K.op(dve, lambda e: e.tensor_copy(t1[:], posi[:]), [pB], [t1B])
        K.op(dve, lambda e: e.tensor_scalar(ang[:], t1[:], misc[:, 0:1], None, op0=ALU.mult), [t1B, cB], [aB])
        for which, dst, dstB in (("sin", sinT, sinB), ("cos", cosT, cosB)):
            off = 0.0 if which == "sin" else 0.25
            K.op(dve, lambda e: e.tensor_scalar(t1[:], ang[:], 1.0 / TWO_PI, off, op0=ALU.mult, op1=ALU.add), [aB], [t1B])
            K.op(dve, lambda e: e.tensor_scalar(t2[:], t1[:], RND, None, op0=ALU.add), [t1B], [t2B])
            K.op(dve, lambda e: e.tensor_scalar(t2[:], t2[:], -RND, None, op0=ALU.add), [t2B], [t2B])
            K.op(dve, lambda e: e.scalar_tensor_tensor(out=t1[:], in0=t2[:], scalar=-CW1, in1=ang[:], op0=ALU.mult, op1=ALU.add), [t2B, aB], [t1B])
            K.op(dve, lambda e: e.scalar_tensor_tensor(out=t1[:], in0=t2[:], scalar=-CW2, in1=t1[:], op0=ALU.mult, op1=ALU.add), [t2B, t1B], [t1B])
            sh = 0.0 if which == "sin" else float(np.pi / 2)
            K.op(dve, lambda e: e.tensor_scalar(t2[:], t1[:], sh, 3.1415925, op0=ALU.add, op1=ALU.min), [t1B], [t2B])
            K.op(dve, lambda e: e.tensor_scalar(t1[:], t2[:], -3.1415925, None, op0=ALU.max), [t2B], [t1B])
            K.op(act, lambda e: e.activation(out=dst[:], in_=t1[:], func=AF.Sin), [t1B], [dstB])
        K.dma(sp, cs_s[0], cosT[:], K.dsem("cso"), reads=[cosB], writes=[DB("cs", 0)])
        K.dma(sp, cs_s[1], sinT[:], K.dsem("cso"), reads=[sinB], writes=[DB("cs", 1)])
        K.barrier()

    def rstd_from_ms(col_ms, col_out, rB):
        K.op(act, lambda e: e.activation(out=stat[:, col_out:col_out + 1], in_=stat[:, col_ms:col_ms + 1],
                                         func=AF.Ln, bias=EPS, scale=1.0), [rB], [rB])
        K.op(act, lambda e: e.activation(out=stat[:, col_out:col_out + 1], in_=stat[:, col_out:col_out + 1],
                                         func=AF.Exp, scale=-0.5), [rB], [rB])

    nstB = [Buf(), Buf()]

    def norm_to_T(xt, xB, gam, gamB, hT, hTB, tok0, h_bfs, hBs, junk, par, pbanks):
        c0 = 20 + 2 * par
        stB = nstB[par]
        h_bf, hB = h_bfs[par], hBs[par]
        K.op(act, lambda e: e.activation(out=junk[:], in_=xt, func=AF.Square, scale=float(D ** -0.5),
                                         accum_out=stat[:, c0:c0 + 1]), [xB], [stB])
        rstd_from_ms(c0, c0 + 1, stB)
        K.op(dve, lambda e: e.scalar_tensor_tensor(out=h_bf[:], in0=xt, scalar=stat[:, c0 + 1:c0 + 2], in1=gam,
                                                   op0=ALU.mult, op1=ALU.mult), [xB, stB, gamB], [hB])
        for gi, k0 in enumerate(range(0, KC, 4)):
            nk = min(4, KC - k0)
            pbk = pbanks[gi % len(pbanks)]
            pb = bf(banks[pbk][:])
            K.pe_group([("tr", pb[:, j * 128:(j + 1) * 128], h_bf[:, (k0 + j) * 128:(k0 + j + 1) * 128], ident_b[:])
                        for j in range(nk)], [hB, cB], [bankb[pbk]])
            src = pb[:, 0:nk * 128].rearrange("p (k t) -> p k t", k=nk)
            dst = hT[:, k0:k0 + nk, tok0:tok0 + 128]
            if gi % 2 == 0:
                K.op(act, lambda e: e.copy(dst, src), [bankb[pbk]], [hTB])
            else:
                K.op(dve, lambda e: e.tensor_copy(dst, src), [bankb[pbk]], [hTB])

    def chk(name):
        if stop == name:
            raise _Stop()

    def _layers():
        chk('rot')
        for l in range(L):
            K.new_layer()
            x_src = x_in if l == 0 else xs
            last = l == L - 1

            with contextlib.ExitStack() as es:
                hT = sb(es, "hT", [128, KC, T], BF16)
                hTB = [Buf() for _ in range(NT)]
                sp_all = sb(es, "sp_all", [128, NT, QKG], F32)
                spB = [Buf() for _ in range(NT)]
                stB = Buf()
                S1 = sb(es, "S1", [128, NKD, 256], F32)
                rdec = sb(es, "rdec", [128, HR * 2 * 128], F32)
                rotB = Buf()
                K.dma(sp, rdec[:], rdec_in, K.dsem("rdec"), writes=[rotB])
                K.op(dve, lambda e: e.memset(S1[:], 0.0), [], S1B)
                NWS = 3
                wring = [sb(es, "wA%d" % i, [128, KC, 512], BF16) for i in range(NWS)]
                wrB = [Buf() for _ in range(NWS)]
                wrds = [K.dsem("wA") for _ in range(NWS)]

                heads = [("g", h, c.o_gq + h * 128, c.o_gk + h * 128, c.o_gv + h * 256, c.o_gg + h * 256, 1) for h in range(HG)]
                heads += [("r", h, c.o_rq + h * 256, c.o_rk + h * 256, c.o_rv + h * 256, c.o_rg + h * 256, 2) for h in range(HR)]

                def load_set(i):
                    if i >= 2 * len(heads):
                        return
                    kind, h, cq, ck_, cv, cg, nblk = heads[i // 2]
                    s = i % NWS
                    if i % 2 == 0:
                        parts = ((cq, 128 * nblk, 0), (ck_, 128 * nblk, 256))
                    else:
                        parts = ((cv, 256, 0), (cg, 256, 256))
                    for (c0, n, dst) in parts:
                        K.dma(pool, wring[s][:, :, dst:dst + n],
                              w_in[l][:, c0:c0 + n].rearrange("(k p) c -> p k c", p=128), wrds[s], writes=[wrB[s]])

                load_set(0)
                load_set(1)

                with contextlib.ExitStack() as es2:
                    gam = sb(es2, "gamA", [128, D], F32)
                    gamB = Buf()
                    xt2 = [sb(es2, "xtA%d" % i, [128, D], F32) for i in range(2)]
                    xtB = [Buf(), Buf()]
                    xds = [K.dsem("xA"), K.dsem("xA")]
                    h_bf = [sb(es2, "h_bfA%d" % i, [128, D], BF16) for i in range(2)]
                    hB = [Buf(), Buf()]
                    junk = sb(es2, "junkA", [128, D], BF16)
                    K.dma(sp, gam[:], an_in[l], K.dsem("gamA"), writes=[gamB])
                    K.dma(sp, xt2[0][:], x_src[0:128, :], xds[0], writes=[xtB[0]])
                    for tt in range(NT):
                        s = tt % 2
                        if tt + 1 < NT:
                            K.dma(sp, xt2[1 - s][:], x_src[(tt + 1) * 128:(tt + 2) * 128, :], xds[1 - s], writes=[xtB[1 - s]])
                        norm_to_T(xt2[s][:], xtB[s], gam[:], gamB, hT, hTB[tt], tt * 128, h_bf, hB, junk, s, [0, 1])
                    K.barrier()
                chk('A0')

                with contextlib.ExitStack() as es2:
                    grT = sb(es2, "grT", [32, T], F32)
                    grB = Buf()
                    gup = sb(es2, "gup", [32, QKG], F32)
                    gupB = Buf()
                    wgr = sb(es2, "wgr", [128, KC, 16], BF16)
                    wgrB = Buf()
                    ez = sb(es2, "ez", [128, QKG], F32)
                    ezB = Buf()
                    K.op(dve, lambda e: e.memset(grT[:], 1.0), [], [grB])
                    K.dma(sp, gup[0:17, :], gup_in[l], K.dsem("gupA"), writes=[gupB])
                    K.dma(pool, wgr[:], w_in[l][:, c.o_gr:c.o_gr + 16].rearrange("(k p) c -> p k c", p=128), K.dsem("wgrA"), writes=[wgrB])
                    for tg in range(0, T, 512):
                        n = min(512, T - tg)
                        tiles = list(range(tg // 128, (tg + n) // 128))
                        K.pe_group([("mm", banks[1][0:16, 0:n], wgr[:, kc, :], hT[:, kc, tg:tg + n], kc == 0, kc == KC - 1)
                                    for kc in range(KC)], [wgrB] + [hTB[t] for t in tiles], [bankb[1]])
                        K.op(act, lambda e: e.copy(grT[0:16, tg:tg + n], banks[1][0:16, 0:n]), [bankb[1]], [grB])
                    for tt in range(NT):
                        K.pe_group([("mm", banks[2][:, 0:QKG], grT[0:17, tt * 128:(tt + 1) * 128], gup[0:17, :], True, True)],
                                   [grB, gupB], [bankb[2]])
                        K.op(act, lambda e: e.activation(out=ez[:], in_=banks[2][:, 0:QKG], func=AF.Exp, scale=-1.0), [bankb[2]], [ezB])
                        K.op(act, lambda e: e.activation(out=sp_all[:, tt, :], in_=ez[:], func=AF.Ln, bias=1.0, scale=1.0), [ezB], [spB[tt]])
                    K.barrier()
                chk('A1')

                cosT = sb(es, "cosT", [128, T], F32)
                sinT = sb(es, "sinT", [128, T], F32)
                K.dma(sp, cosT[:], cs_s[0], K.dsem("cos"), reads=[DB("cs", 0)], writes=[rotB])
                K.dma(sp, sinT[:], cs_s[1], K.dsem("sin"), reads=[DB("cs", 1)], writes=[rotB])
                eb = sb(es, "eb", [128, 512], F32)
                enb = sb(es, "enb", [128, 512], F32)
                ebB, enbB = Buf(), Buf()
                qf = [sb(es, "qf%d" % i, [128, 512], F32) for i in range(2)]
                qfB = [Buf(), Buf()]
                rt = [sb(es, "rtA%d" % i, [128, 512], F32) for i in range(4)]
                rtB = [Buf() for _ in range(4)]
                fm = [sb(es, "fm%d" % i, [128, 512], BF16) for i in range(4)]
                fmB = [Buf() for _ in range(4)]
                fmds = [K.dsem("fm") for _ in range(4)]
                kdt = [sb(es, "kdt%d" % i, [128, 256], BF16) for i in range(4)]
                kdtB = [Buf() for _ in range(4)]
                kdds = [K.dsem("kd") for _ in range(4)]
                vt = [sb(es, "vt%d" % i, [128, 256], BF16) for i in range(4)]
                vtB = [Buf() for _ in range(4)]
                vds = [K.dsem("v") for _ in range(4)]
                sgt = [sb(es, "sgt%d" % i, [128, 256], F32) for i in range(4)]
                sgtB = [Buf() for _ in range(4)]
                sgds = [K.dsem("sg") for _ in range(4)]

                fmi = 0
                tok_i = [0]
                for hi, (kind, h, cq, ck, cv, cg, nblk) in enumerate(heads):
                    if hi == 1:
                        chk('A2f')
                    load_set(2 * hi + 2)
                    wt, wtB = wring[(2 * hi) % NWS], wrB[(2 * hi) % NWS]
                    wv, wvB = wring[(2 * hi + 1) % NWS], wrB[(2 * hi + 1) % NWS]
                    sblk0 = h if kind == "g" else HG + 2 * h
                    qblk0 = 2 * h if kind == "g" else 2 * HG + 4 * h
                    for tg in range(0, T, 512):
                        n = min(512, T - tg)
                        tiles = list(range(tg // 128, (tg + n) // 128))
                        hr = [hTB[t] for t in tiles]
                        if kind == "g":
                            K.pe_group([("mm", banks[1][:, j * 128:(j + 1) * 128], sp_all[:, t, h * 128:(h + 1) * 128], tri_f[:], True, True)
                                        for j, t in enumerate(tiles)], [spB[t] for t in tiles] + [cB], [bankb[1]])
                            K.op(act, lambda e: e.activation(out=eb[:, 0:n], in_=banks[1][:, 0:n], func=AF.Exp), [bankb[1]], [ebB])
                            K.op(act, lambda e: e.activation(out=enb[:, 0:n], in_=banks[1][:, 0:n], func=AF.Exp, scale=-1.0), [bankb[1]], [enbB])
                            for j, t in enumerate(tiles):
                                K.op(dve, lambda e: e.tensor_copy(d_all[:, h * NT + t:h * NT + t + 1], eb[:, j * 128 + 127:j * 128 + 128]), [ebB], [dB])
                        chk('A2a')
                        for qk in range(2):
                            wcol = 0 if qk == 0 else 256
                            for blk in range(nblk):
                                bk = 2 + (qk * 2 + blk) % 2 if kind == "g" else 2 + blk
                                pbk = banks[bk]
                                K.pe_group([("mm", pbk[:, 0:n], wt[:, kc, wcol + blk * 128:wcol + (blk + 1) * 128], hT[:, kc, tg:tg + n], kc == 0, kc == KC - 1)
                                            for kc in range(KC)], [wtB] + hr, [bankb[bk]])
                                if kind == "g":
                                    f = fmi % 4
                                    fmi += 1
                                    if qk == 0:
                                        K.op(dve, lambda e: e.scalar_tensor_tensor(out=fm[f][:, 0:n], in0=pbk[:, 0:n], scalar=float(128 ** -0.5), in1=eb[:, 0:n],
                                                                                   op0=ALU.mult, op1=ALU.mult), [bankb[bk], ebB], [fmB[f]])
                                    else:
                                        K.op(dve, lambda e: e.tensor_tensor(out=fm[f][:, 0:n], in0=pbk[:, 0:n], in1=enb[:, 0:n], op=ALU.mult), [bankb[bk], enbB], [fmB[f]])
                                    K.dma(sp, qk_s[:, qblk0 + qk, tg:tg + n], fm[f][:, 0:n], fmds[f], reads=[fmB[f]], writes=[DB("qk", qblk0 + qk, tg)])
                                    if qk == 1:
                                        kf = [f]
                                else:
                                    dec = rdec[:, (h * 2 + qk) * 128:(h * 2 + qk + 1) * 128]
                                    for j in range(n // 128):
                                        K.op(dve, lambda e: e.tensor_tensor(out=qf[blk][:, j * 128:(j + 1) * 128], in0=pbk[:, j * 128:(j + 1) * 128], in1=dec, op=ALU.mult),
                                             [bankb[bk], rotB], [qfB[blk]])
                            if kind == "r":
                                fs = []
                                for blk in range(2):
                                    a, b_ = (qf[0], qf[1]) if blk == 0 else (qf[1], qf[0])
                                    aB_, bB_ = (qfB[0], qfB[1]) if blk == 0 else (qfB[1], qfB[0])
                                    r0, r1 = rt[2 * blk], rt[2 * blk + 1]
                                    r0B, r1B = rtB[2 * blk], rtB[2 * blk + 1]
                                    K.op(pool, lambda e: e.tensor_tensor(out=r0[:, 0:n], in0=a[:, 0:n], in1=cosT[:, tg:tg + n], op=ALU.mult), [aB_, rotB], [r0B])
                                    K.op(pool, lambda e: e.tensor_tensor(out=r1[:, 0:n], in0=b_[:, 0:n], in1=sinT[:, tg:tg + n], op=ALU.mult), [bB_, rotB], [r1B])
                                    f = fmi % 4
                                    fmi += 1
                                    K.op(dve, lambda e: e.tensor_tensor(out=fm[f][:, 0:n], in0=r0[:, 0:n], in1=r1[:, 0:n],
                                                                        op=(ALU.subtract if blk == 0 else ALU.add)), [r0B, r1B], [fmB[f]])
                                    bi = qblk0 + qk * 2 + blk
                                    K.dma(sp, qk_s[:, bi, tg:tg + n], fm[f][:, 0:n], fmds[f], reads=[fmB[f]], writes=[DB("qk", bi, tg)])
                                    fs.append(f)
                                if qk == 1:
                                    kf = fs
                        if tg + 512 >= T:
                            load_set(2 * hi + 3)
                        chk('A2b')
                        vcol = (h * 256) if kind == "g" else (VG + h * 256)
                        slot = {}

                        def e_vg(j, t):
                            si = tok_i[0] % 4
                            bk = (5, 6, 0)[tok_i[0] % 3]
                            tok_i[0] += 1
                            slot[j] = si
                            K.pe_group([("mm", banks[bk][:, :], hT[:, kc, t * 128:(t + 1) * 128], wv[:, kc, 0:512], kc == 0, kc == KC - 1) for kc in range(KC)],
                                       [wvB, hTB[t]], [bankb[bk]])
                            K.op(dve, lambda e: e.tensor_copy(vt[si][:], banks[bk][:, 0:256]), [bankb[bk]], [vtB[si]])
                            K.op(act, lambda e: e.activation(out=sgt[si][:], in_=banks[bk][:, 256:512], func=AF.Silu), [bankb[bk]], [sgtB[si]])
                            K.dma(sp, v_s[t * 128:(t + 1) * 128, vcol:vcol + 256], vt[si][:], vds[si], reads=[vtB[si]], writes=[DB("v", vcol, t)])
                            K.dma(sp, sg_s[t * 128:(t + 1) * 128, vcol:vcol + 256], sgt[si][:], sgds[si], reads=[sgtB[si]], writes=[DB("sg", vcol, t)])

                        def e_trl(j, t):
                            si = slot[j]
                            pb = bf(banks[4][:])
                            K.pe_group([("tr", pb[:, b2 * 128:(b2 + 1) * 128], fm[kf[b2]][:, j * 128:(j + 1) * 128], ident_b[:]) for b2 in range(nblk)],
                                       [fmB[x] for x in kf] + [cB], [bankb[4]])
                            K.op(act, lambda e: e.copy(kdt[si][:, 0:nblk * 128], pb[:, 0:nblk * 128]), [bankb[4]], [kdtB[si]])
                            K.dma(sp, kd_s[t * 128:(t + 1) * 128, sblk0 * 128:(sblk0 + nblk) * 128], kdt[si][:, 0:nblk * 128], kdds[si],
                                  reads=[kdtB[si]], writes=[DB("kd", sblk0, t)])

                        def e_local(j, t):
                            si = slot[j]
                            for b2 in range(nblk):
                                K.pe_group([("mm", banks[7][:, b2 * 256:(b2 + 1) * 256], kdt[si][:, b2 * 128:(b2 + 1) * 128], vt[si][:], True, True)],
                                           [kdtB[si], vtB[si]], [bankb[7]])
                            for b2 in range(nblk):
                                sbk = sblk0 + b2
                                dsc = d_all[:, h * NT + t:h * NT + t + 1] if kind == "g" else float(hgam[h] ** 128)
                                K.op(dve, lambda e: e.tensor_scalar(S1[:, sbk, :], S1[:, sbk, :], dsc, None, op0=ALU.mult), [S1B[sbk], dB], [S1B[sbk]])
                                K.op(dve, lambda e: e.scalar_tensor_tensor(out=S1[:, sbk, :], in0=banks[7][:, b2 * 256:(b2 + 1) * 256], scalar=dsc, in1=S1[:, sbk, :],
                                                                           op0=ALU.mult, op1=ALU.add), [bankb[7], S1B[sbk], dB], [S1B[sbk]])

                        AHEAD = 3
                        ntl = len(tiles)
                        for j in range(min(AHEAD, ntl)):
                            e_vg(j, tiles[j])
                        for j in range(ntl):
                            e_trl(j, tiles[j])
                            if j + AHEAD < ntl:
                                e_vg(j + AHEAD, tiles[j + AHEAD])
                            e_local(j, tiles[j])
                chk('A2')
                ccds = K.dsem("cc")
                ccB, ccoB = Buf(), Buf()
                K.dma(sp, cc_in_t[l].ap().rearrange("(b p) v -> p b v", p=128), S1[:], ccds, reads=S1B, writes=[ccB])
                K.barrier()

            chk('A')
            with contextlib.ExitStack() as es:
                S1 = sb(es, "S1b", [128, NKD, 256], F32)
                Sb = sb(es, "Sb", [128, NKD, 256], BF16)
                wo = sb(es, "wo", [128, KM, D], BF16)
                woB = Buf()
                gon = sb(es, "gon", [128, VG], F32)
                rgn = sb(es, "rgn", [128, VR], F32)
                rbn = sb(es, "rbn", [128, VR], F32)
                nB = Buf()
                qk2 = [sb(es, "qkB%d" % i, [128, NB, 128], BF16) for i in range(2)]
                kd2 = [sb(es, "kdB%d" % i, [128, NKD * 128], BF16) for i in range(2)]
                v2 = [sb(es, "vB%d" % i, [128, DMIX], BF16) for i in range(2)]
                sg2 = [sb(es, "sgB%d" % i, [128, DMIX], F32) for i in range(2)]
                x2 = [sb(es, "xB%d" % i, [128, D], F32) for i in range(2)]
                inB = [Buf(), Buf()]
                xB = [Buf(), Buf()]
                inds = [K.dsem("inB"), K.dsem("inB")]
                xds = [K.dsem("xB"), K.dsem("xB")]
                xods = [K.dsem("xoB"), K.dsem("xoB")]
                sc = [sb(es, "sc%d" % i, [128, 128], BF16) for i in range(2)]
                scB = [Buf(), Buf()]
                tn = [sb(es, "tn%d" % i, [128, 256], F32) for i in range(2)]
                tnB = [Buf(), Buf()]
                junk = sb(es, "junkB", [128, 256], BF16)
                mix = [sb(es, "mix%d" % i, [128, DMIX], BF16) for i in range(2)]
                mixB = [Buf(), Buf()]
                mixT = sb(es, "mixT", [128, KM, 128], BF16)
                mixTB = Buf()
                stB = Buf()

                cds = K.dsem("cB")
                for g4 in range(0, D, 512):
                    n4 = min(512, D - g4)
                    K.dma(pool, wo[:, :, g4:g4 + n4], w_out[l][:, g4:g4 + n4].rearrange("(k p) c -> p k c", p=128), cds, writes=[woB])
                K.dma(sp, gon[:], gon_in[l], cds, writes=[nB])
                K.dma(sp, rgn[:], rng_in[l], cds, writes=[nB])
                K.dma(sp, rbn[:], rnb_in[l], cds, writes=[nB])
                if use_cc:
                    ccsem = nc.alloc_semaphore("ccs%d" % l)
                    nc.gpsimd.collective_compute("AllGather", ALU.bypass, replica_groups=pair_groups,
                                                 ins=[cc_in_t[l].ap().opt()], outs=[cc_out_t[l].ap().opt()]).then_inc(ccsem)
                    nc.gpsimd.wait_ge(ccsem, 1)
                    src = cc_out_t[l].ap()[0:NKD * 128, :]
                else:
                    src = cc_in_t[l].ap()
                K.dma(pool, S1[:], src.rearrange("(b p) v -> p b v", p=128), K.dsem("ccl"), reads=[ccB], writes=S1B)
                K.op(dve, lambda e: e.tensor_scalar(S1[:], S1[:], misc[:, 1:2], None, op0=ALU.mult), S1B + [cB], S1B)
                K.op(dve, lambda e: e.tensor_copy(Sb[:], S1[:]), S1B, SbB)

                def load_tile(t):
                    s = t % 2
                    tk = slice(t * 128, (t + 1) * 128)
                    K.dma(sp, qk2[s][:], qk_s[:, :, tk], inds[s], reads=[DB("qk", b_, (t // 4) * 512) for b_ in range(NB)], writes=[inB[s]])
                    K.dma(sp, kd2[s][:], kd_s[tk, :], inds[s], reads=[DB("kd", b_, t) for b_ in range(NKD)], writes=[inB[s]])
                    K.dma(sp, v2[s][:], v_s[tk, :], inds[s], reads=[DB("v", cc_, t) for cc_ in range(0, DMIX, 256)], writes=[inB[s]])
                    K.dma(sp, sg2[s][:], sg_s[tk, :], inds[s], reads=[DB("sg", cc_, t) for cc_ in range(0, DMIX, 256)], writes=[inB[s]])

                def load_x(t):
                    s = t % 2
                    tk = slice(t * 128, (t + 1) * 128)
                    K.dma(sp, x2[s][:], x_src[tk, :], xds[s], reads=[DB("xs", t)] if l > 0 else [], writes=[xB[s]])

                hi_all = [("g", h) for h in range(HG)] + [("r", h) for h in range(HR)]
                NHh = len(hi_all)

                def hinfo(idx):
                    kind, h = hi_all[idx]
                    nblk = 1 if kind == "g" else 2
                    sblk0 = h if kind == "g" else HG + 2 * h
                    qb0 = 2 * h if kind == "g" else 2 * HG + 4 * h
                    vcol = (h * 256) if kind == "g" else (VG + h * 256)
                    return kind, h, nblk, sblk0, qb0, vcol, idx % 2

                def e_scores(t, idx):
                    kind, h, nblk, sblk0, qb0, vcol, i2 = hinfo(idx)
                    s = t % 2
                    K.pe_group([("mm", banks[i2][:, 0:128], qk2[s][:, qb0 + nblk + b2, :], qk2[s][:, qb0 + b2, :], b2 == 0, b2 == nblk - 1) for b2 in range(nblk)],
                               [inB[s]], [bankb[i2]])
                    K.op(dve, lambda e: e.tensor_tensor(out=sc[i2][:], in0=banks[i2][:, 0:128], in1=mask_f[:], op=ALU.mult), [bankb[i2], cB], [scB[i2]])

                def e_head(t, idx):
                    kind, h, nblk, sblk0, qb0, vcol, i2 = hinfo(idx)
                    s = t % 2
                    pbo, pbl = 2 + i2, 4 + i2
                    items = [("mm", banks[pbo][:, 0:256], sc[i2][:], v2[s][:, vcol:vcol + 256], True, False)]
                    for b2 in range(nblk):
                        items.append(("mm", banks[pbo][:, 0:256], qk2[s][:, qb0 + b2, :], Sb[:, sblk0 + b2, :], False, b2 == nblk - 1))
                    K.pe_group(items, [scB[i2], inB[s]] + [SbB[sblk0 + b2] for b2 in range(nblk)], [bankb[pbo]])
                    for b2 in range(nblk):
                        K.pe_group([("mm", banks[pbl][:, b2 * 256:(b2 + 1) * 256], kd2[s][:, (sblk0 + b2) * 128:(sblk0 + b2 + 1) * 128], v2[s][:, vcol:vcol + 256], True, True)],
                                   [inB[s]], [bankb[pbl]])
                    for b2 in range(nblk):
                        sbk = sblk0 + b2
                        if kind == "g":
                            dsc = d_all[:, h * NT + t:h * NT + t + 1]
                        else:
                            dsc = float(hgam[h] ** 128)
                        K.op(act, lambda e: e.activation(out=S1[:, sbk, :], in_=S1[:, sbk, :], func=AF.Identity, scale=dsc), [S1B[sbk], dB], [S1B[sbk]])
                        K.op(dve, lambda e: e.scalar_tensor_tensor(out=S1[:, sbk, :], in0=banks[pbl][:, b2 * 256:(b2 + 1) * 256], scalar=dsc, in1=S1[:, sbk, :],
                                                                   op0=ALU.mult, op1=ALU.add), [bankb[pbl], S1B[sbk], dB], [S1B[sbk]])
                        K.op(pool, lambda e: e.tensor_copy(Sb[:, sbk, :], S1[:, sbk, :]), [S1B[sbk]], [SbB[sbk]])
                    po = banks[pbo][:, 0:256]
                    c0 = 2 + 6 * i2
                    if kind == "g":
                        K.op(act, lambda e: e.activation(out=junk[:], in_=po, func=AF.Square, scale=float(256 ** -0.5), accum_out=stat[:, c0:c0 + 1]), [bankb[pbo]], [stB])
                        rstd_from_ms(c0, c0 + 1, stB)
                        K.op(dve, lambda e: e.scalar_tensor_tensor(out=tn[i2][:], in0=po, scalar=stat[:, c0 + 1:c0 + 2], in1=gon[:, h * 256:(h + 1) * 256],
                                                                   op0=ALU.mult, op1=ALU.mult), [bankb[pbo], stB, nB], [tnB[i2]])
                    else:
                        K.op(act, lambda e: e.activation(out=junk[:], in_=po, func=AF.Identity, scale=float(1.0 / 256), accum_out=stat[:, c0:c0 + 1]), [bankb[pbo]], [stB])
                        K.op(act, lambda e: e.activation(out=junk[:], in_=po, func=AF.Square, scale=float(256 ** -0.5), accum_out=stat[:, c0 + 1:c0 + 2]), [bankb[pbo]], [stB])
                        K.op(dve, lambda e: e.tensor_tensor(out=stat[:, c0 + 2:c0 + 3], in0=stat[:, c0:c0 + 1], in1=stat[:, c0:c0 + 1], op=ALU.mult), [stB], [stB])
                        K.op(dve, lambda e: e.tensor_tensor(out=stat[:, c0 + 3:c0 + 4], in0=stat[:, c0 + 1:c0 + 2], in1=stat[:, c0 + 2:c0 + 3], op=ALU.subtract), [stB], [stB])
                        rstd_from_ms(c0 + 3, c0 + 4, stB)
                        K.op(dve, lambda e: e.scalar_tensor_tensor(out=tn[i2][:], in0=po, scalar=stat[:, c0:c0 + 1], in1=rgn[:, h * 256:(h + 1) * 256],
                                                                   op0=ALU.subtract, op1=ALU.mult), [bankb[pbo], stB, nB], [tnB[i2]])
                        K.op(dve, lambda e: e.scalar_tensor_tensor(out=tn[i2][:], in0=tn[i2][:], scalar=stat[:, c0 + 4:c0 + 5], in1=rbn[:, h * 256:(h + 1) * 256],
                                                                   op0=ALU.mult, op1=ALU.add), [tnB[i2], stB, nB], [tnB[i2]])
                    K.op(pool, lambda e: e.tensor_tensor(out=mix[s][:, vcol:vcol + 256], in0=tn[i2][:], in1=sg2[s][:, vcol:vcol + 256], op=ALU.mult), [tnB[i2], inB[s]], [mixB[s]])

                def o_pieces(t):
                    s = t % 2
                    pl = []
                    for k0 in range(0, KM, 4):
                        def tr_piece(k0=k0):
                            nk = min(4, KM - k0)
                            pb = bf(banks[6][:])
                            K.pe_group([("tr", pb[:, j * 128:(j + 1) * 128], mix[s][:, (k0 + j) * 128:(k0 + j + 1) * 128], ident_b[:]) for j in range(nk)], [mixB[s], cB], [bankb[6]])
                            K.op(act, lambda e: e.copy(mixT[:, k0:k0 + nk, :], pb[:, 0:nk * 128].rearrange("p (k t) -> p k t", k=nk)), [bankb[6]], [mixTB])
                        pl.append(tr_piece)
                    ng = len(range(0, D, 512))
                    for gi, g4 in enumerate(range(0, D, 512)):
                        def op_piece(gi=gi, g4=g4):
                            n = min(512, D - g4)
                            K.pe_group([("mm", banks[7][:, 0:n], mixT[:, km, :], wo[:, km, g4:g4 + n], km == 0, km == KM - 1) for km in range(KM)], [mixTB, woB], [bankb[7]])
                            K.op(dve, lambda e: e.tensor_tensor(out=x2[s][:, g4:g4 + n], in0=banks[7][:, 0:n], in1=x2[s][:, g4:g4 + n], op=ALU.add), [bankb[7], xB[s]], [xB[s]])
                            if gi == ng - 1:
                                K.dma(sp, xs[t * 128:(t + 1) * 128, :], x2[s][:], xods[s], reads=[xB[s]], writes=[DB("xs", t)])
                        pl.append(op_piece)
                    return pl

                load_tile(0)
                for t in range(NT + 1):
                    if t + 1 < NT:
                        load_tile(t + 1)
                    if t < NT:
                        load_x(t)
                    pl = o_pieces(t - 1) if t >= 1 else []
                    npc = len(pl)
                    if t < NT:
                        e_scores(t, 0)
                        for idx in range(NHh):
                            if idx + 1 < NHh:
                                e_scores(t, idx + 1)
                            e_head(t, idx)
                            for pc in pl[idx * npc // NHh:(idx + 1) * npc // NHh]:
                                pc()
                    else:
                        for pc in pl:
                            pc()
                K.barrier()

            chk('B1')
            with contextlib.ExitStack() as es:
                TBF = min(c.TBF, T)
                NTB = TBF // 128
                FG = 512
                gam = sb(es, "gamF", [128, D], F32)
                gamB = Buf()
                xg = sb(es, "xg", [128, NTB, D], F32)
                xgB = [Buf() for _ in range(NTB)]
                xgds = [K.dsem("xg") for _ in range(NTB)]
                xods = [K.dsem("xgo") for _ in range(NTB)]
                h2T = sb(es, "h2T", [128, KC, TBF], BF16)
                h2TB = [Buf() for _ in range(NTB)]
                h_bf = [sb(es, "h_bfF%d" % i, [128, D], BF16) for i in range(2)]
                hB = [Buf(), Buf()]
                junk = sb(es, "junkF", [128, D], BF16)
                stB = Buf()
                wu = [sb(es, "wu%d" % i, [128, KC, FG], BF16) for i in range(2)]
                wuB = [Buf(), Buf()]
                wuds = [K.dsem("wu"), K.dsem("wu")]
                wd = [sb(es, "wd%d" % i, [128, FG // 128, D], BF16) for i in range(2)]
                wdB = [Buf(), Buf()]
                wdds = [K.dsem("wd"), K.dsem("wd")]
                uT = [sb(es, "uT%d" % i, [128, FG // 128, TBF], BF16) for i in range(2)]
                uTB = [Buf(), Buf()]
                sq = [sb(es, "sq%d" % i, [128, 512], F32) for i in range(2)]
                sqB = [Buf(), Buf()]
                gds = K.dsem("gF")
                NFG = DFF // FG
                nload = [0]

                def load_w(fi, bi=0):
                    s = (bi * NFG + fi) % 2
                    K.dma(pool, wu[s][:], w_up[l][:, fi * FG:(fi + 1) * FG].rearrange("(k p) c -> p k c", p=128), wuds[s], writes=[wuB[s]])
                    K.dma(pool, wd[s][:], w_down[l][fi * FG:(fi + 1) * FG, :].rearrange("(k p) c -> p k c", p=128), wdds[s], writes=[wdB[s]])

                def load_xg(tb_, j):
                    t_ = tb_ // 128 + j
                    K.dma(sp, xg[:, j, :], xs[t_ * 128:(t_ + 1) * 128, :], xgds[j], reads=[DB("xs", t_)], writes=[xgB[j]])

                nblk_f = T // TBF
                for bi, tb in enumerate(range(0, T, TBF)):
                    has_next = bi + 1 < nblk_f
                    if bi == 0:
                        load_w(0)
                        for j in range(NTB):
                            load_xg(tb, j)
                    if bi == 0 or last:
                        K.dma(sp, gam[:], mn_in[l], gds, writes=[gamB])
                    ev = 0
                    for fi in range(NFG):
                        s = (bi * NFG + fi) % 2
                        if fi + 1 < NFG:
                            load_w(fi + 1, bi)
                        elif has_next:
                            load_w(0, bi + 1)
                        for tg in range(0, TBF, 512):
                            n = min(512, TBF - tg)
                            if fi == 0:
                                for j in range(tg // 128, (tg + n) // 128):
                                    norm_to_T(xg[:, j, :], xgB[j], gam[:], gamB, h2T, h2TB[j], j * 128, h_bf, hB, junk, j % 2, [0, 7])
                            for fc in range(FG // 128):
                                e2 = ev % 2
                                ev += 1
                                bk = 1 + e2
                                K.pe_group([("mm", banks[bk][:, 0:n], wu[s][:, kc, fc * 128:(fc + 1) * 128], h2T[:, kc, tg:tg + n], kc == 0, kc == KC - 1) for kc in range(KC)],
                                           [wuB[s]] + h2TB[tg // 128:(tg + n) // 128], [bankb[bk]])
                                K.op(act, lambda e: e.activation(out=sq[e2][:, 0:n], in_=banks[bk][:, 0:n], func=AF.Square), [bankb[bk]], [sqB[e2]])
                                K.op(dve, lambda e: e.scalar_tensor_tensor(out=uT[s][:, fc, tg:tg + n], in0=banks[bk][:, 0:n], scalar=0.0, in1=sq[e2][:, 0:n],
                                                                           op0=ALU.is_gt, op1=ALU.mult), [bankb[bk], sqB[e2]], [uTB[s]])
                        di = 0
                        for j in range(NTB):
                            for g4 in range(0, D, 512):
                                n = min(512, D - g4)
                                bk = 3 + di % 4
                                di += 1
                                K.pe_group([("mm", banks[bk][:, 0:n], uT[s][:, fc, j * 128:(j + 1) * 128], wd[s][:, fc, g4:g4 + n], fc == 0, fc == FG // 128 - 1) for fc in range(FG // 128)],
                                           [uTB[s], wdB[s]], [bankb[bk]])
                                K.op(dve, lambda e: e.tensor_tensor(out=xg[:, j, g4:g4 + n], in0=banks[bk][:, 0:n], in1=xg[:, j, g4:g4 + n], op=ALU.add), [bankb[bk], xgB[j]], [xgB[j]])
                            if fi == NFG - 1 and not last:
                                t = tb // 128 + j
                                K.dma(sp, xs[t * 128:(t + 1) * 128, :], xg[:, j, :], xods[j], reads=[xgB[j]], writes=[DB("xs", t)])
                                if has_next:
                                    load_xg(tb + TBF, j)
                    if last:
                        K.dma(sp, gam[:], fn_in, gds, writes=[gamB])
                    for j in range(NTB):
                        t = tb // 128 + j
                        if not last:
                            pass
                        else:
                            K.op(act, lambda e: e.activation(out=junk[:], in_=xg[:, j, :], func=AF.Square, scale=float(D ** -0.5), accum_out=stat[:, 0:1]), [xgB[j]], [stB])
                            rstd_from_ms(0, 1, stB)
                            K.op(dve, lambda e: e.scalar_tensor_tensor(out=xg[:, j, :], in0=xg[:, j, :], scalar=stat[:, 1:2], in1=gam[:], op0=ALU.mult, op1=ALU.mult),
                                 [xgB[j], stB, gamB], [xgB[j]])
                            K.dma(sp, out[t * 128:(t + 1) * 128, :], xg[:, j, :], xods[j], reads=[xgB[j]], writes=[DB("out", t)])
                            if has_next:
                                load_xg(tb + TBF, j)
                K.barrier()

    try:
        _layers()
        K.barrier()
        es0.close()
    except _Stop:
        K.barrier()
    return nc


def make_consts(cfg):
    HR = cfg.HR
    ident = np.eye(128, dtype=np.float32)
    j = np.arange(128)[:, None]
    i = np.arange(128)[None, :]
    causal = (j <= i)
    tri = np.where(causal, -1.0 / 16.0, 0.0).astype(np.float32)
    mask = causal.astype(np.float32)
    rdec = np.zeros((128, HR, 2, 128), dtype=np.float64)
    pos = np.arange(128, dtype=np.float64)
    for h in range(HR):
        lg = np.log1p(-(2.0 ** (-5.0 - h)))
        rdec[:, h, 0, :] = np.exp(lg * (pos + 1.0))[None, :]
        rdec[:, h, 1, :] = (np.exp(-lg * (pos + 1.0)) * (256.0 ** -0.5))[None, :]
    rdec = rdec.reshape(128, HR * 2 * 128).astype(np.float32)
    inv_freq = (np.float32(ROPE_BASE) ** (-np.arange(0, 256, 2, dtype=np.float32) / np.float32(256))).astype(np.float32)
    return ident, tri, mask, rdec, inv_freq


def make_in_maps(cfg, n_cores, x, positions, attn_norm, w_in, gla_gate_up, gla_gate_bias, gla_out_norm,
                 ret_norm_gain, ret_norm_bias, w_out, mlp_norm, w_up, w_down, final_norm):
    f = np.float32
    T = cfg.T
    ident, tri, mask, rdec, inv_freq = make_consts(cfg)
    bc = lambda a: np.ascontiguousarray(np.broadcast_to(np.asarray(a, f)[:, None, :], (a.shape[0], 128, a.shape[1])))
    gup = np.ascontiguousarray(np.concatenate([np.asarray(gla_gate_up, f), np.asarray(gla_gate_bias, f)[:, None, :]], axis=1))
    shared = {
        "w_in": np.ascontiguousarray(w_in, dtype=f), "gup": gup, "an": bc(attn_norm), "mn": bc(mlp_norm),
        "fn": np.ascontiguousarray(np.broadcast_to(np.asarray(final_norm, f)[None, :], (128, final_norm.shape[0]))),
        "gon": bc(gla_out_norm), "rng": bc(ret_norm_gain), "rnb": bc(ret_norm_bias),
        "w_out": np.ascontiguousarray(w_out, dtype=f), "w_up": np.ascontiguousarray(w_up, dtype=f),
        "w_down": np.ascontiguousarray(w_down, dtype=f),
        "ident": ident, "tri": tri, "mask": mask, "rdec": rdec,
    }
    x = np.asarray(x, f)
    positions = np.asarray(positions, np.int32)
    maps = []
    for cid in range(n_cores):
        b, half = cid // 2, cid % 2
        m = dict(shared)
        m["x"] = np.ascontiguousarray(x[b, half * T:(half + 1) * T, :])
        m["pos"] = np.ascontiguousarray(np.broadcast_to(positions[b, half * T:(half + 1) * T][None, :], (128, T)))
        misc = np.zeros((128, 4), f)
        misc[:, 0] = inv_freq
        misc[:, 1] = float(half)
        m["misc"] = misc
        maps.append(m)
    return maps


def kernel(x, positions, attn_norm, w_in, gla_gate_up, gla_gate_bias, gla_out_norm,
           ret_norm_gain, ret_norm_bias, w_out, mlp_norm, w_up, w_down, final_norm):
    cfg = Cfg()
    n = 8
    B, S, D = x.shape
    nc = build(cfg, use_cc=True, pair_groups=[[0, 1], [2, 3], [4, 5], [6, 7]])
    maps = make_in_maps(cfg, n, x, positions, attn_norm, w_in, gla_gate_up, gla_gate_bias, gla_out_norm,
                        ret_norm_gain, ret_norm_bias, w_out, mlp_norm, w_up, w_down, final_norm)
    res = run_bass_kernel_spmd(nc, maps, core_ids=list(range(n)))
    outp = np.empty((B, S, D), np.float32)
    for cid in range(n):
        b, half = cid // 2, cid % 2
        outp[b, half * cfg.T:(half + 1) * cfg.T, :] = res.results[cid]["out"]
    return outp
```

```python
import contextlib
import numpy as np
import concourse.bass as bass
import concourse.mybir as mybir
from concourse.bass_utils import run_bass_kernel_spmd

F32 = mybir.dt.float32
BF16 = mybir.dt.bfloat16
I32 = mybir.dt.int32
AF = mybir.ActivationFunctionType
ALU = mybir.AluOpType

EPS = 1e-6
ROPE_BASE = 10000.0
TWO_PI = 2.0 * np.pi
CW1 = float(np.float32(6.28125))
CW2 = float(np.float32(TWO_PI - 6.28125))
RND = 12582912.0


class Cfg:
    def __init__(self, D=2048, T=2048, HG=4, HR=4, DFF=8192, L=2, TBF=1024):
        self.D, self.T, self.HG, self.HR, self.DFF, self.L, self.TBF = D, T, HG, HR, DFF, L, TBF
        self.KC = D // 128
        self.NT = T // 128
        self.QKG, self.VG = HG * 128, HG * 256
        self.QKR, self.VR = HR * 256, HR * 256
        self.DMIX = self.VG + self.VR
        self.KM = self.DMIX // 128
        self.o_gq = 0
        self.o_gk = self.QKG
        self.o_gv = 2 * self.QKG
        self.o_gg = self.o_gv + self.VG
        self.o_gr = self.o_gg + self.VG
        self.o_rq = self.o_gr + 16
        self.o_rk = self.o_rq + self.QKR
        self.o_rv = self.o_rk + self.QKR
        self.o_rg = self.o_rv + self.VR
        self.DIN = self.o_rg + self.VR
        self.NB = 2 * HG + 4 * HR
        self.NKD = HG + 2 * HR
        self.NH = HG + HR


class Buf:
    __slots__ = ("w", "r", "excl")

    def __init__(self, excl=False):
        self.w = {}
        self.r = {}
        self.excl = excl


class Eng:
    def __init__(self, ctx, name, handle):
        self.h = handle
        self.key = "e_" + name
        self.sem = ctx.nc.alloc_semaphore("es_" + name)
        ctx.sems[self.key] = self.sem
        self.cnt = 0
        self.waited = {}


class DSem:
    def __init__(self, ctx, name):
        self.ctx = ctx
        self.name = name
        self.sub = {}

    def for_queue(self, q):
        d = self.sub.get(q.key)
        if d is None:
            d = _DSub(self.ctx, self.name + "_" + q.key)
            self.sub[q.key] = d
            self.ctx.dsubs.append(d)
        return d


class _DSub:
    def __init__(self, ctx, name):
        self.key = "d_" + name
        self.sem = ctx.nc.alloc_semaphore("ds_" + name)
        ctx.sems[self.key] = self.sem
        self.cnt = 0


def _merge(d, s):
    for k, v in s.items():
        if d.get(k, 0) < v:
            d[k] = v


class Ctx:
    def __init__(self, nc):
        self.nc = nc
        self.sems = {}
        self.pe = Eng(self, "pe", nc.tensor)
        self.act = Eng(self, "act", nc.scalar)
        self.dve = Eng(self, "dve", nc.vector)
        self.pool = Eng(self, "pool", nc.gpsimd)
        self.sp = Eng(self, "sp", nc.sync)
        self.engs = [self.pe, self.act, self.dve, self.pool, self.sp]
        self.dsems = []
        self.dsubs = []
        self.dnames = {}
        self.dcache = {}

    def dsem(self, name):
        i = self.dnames.get(name, 0)
        self.dnames[name] = i + 1
        key = "%s_%d" % (name, i)
        d = self.dcache.get(key)
        if d is None:
            d = self.dcache[key] = DSem(self, key)
            self.dsems.append(d)
        return d

    def new_layer(self):
        self.dnames = {}

    def _wait(self, eng, deps):
        for key, val in deps.items():
            if eng.waited.get(key, 0) >= val:
                continue
            eng.h.wait_ge(self.sems[key], val)
            eng.waited[key] = val

    def _deps(self, eng, reads, writes):
        if any(b.excl for b in reads):
            writes = list(writes) + [b for b in reads if b.excl]
            reads = [b for b in reads if not b.excl]
        d = {}
        for b in reads:
            _merge(d, b.w)
        for b in writes:
            w = dict(b.w)
            r = dict(b.r)
            _merge(d, w)
            _merge(d, r)
        if eng is self.pe:
            d.pop(eng.key, None)
        return d

    def _mark(self, key, cnt, reads, writes):
        if any(b.excl for b in reads):
            writes = list(writes) + [b for b in reads if b.excl]
            reads = [b for b in reads if not b.excl]
        for b in writes:
            b.w = {key: cnt}
            b.r = {}
        for b in reads:
            if not any(b is x for x in writes):
                if b.r.get(key, 0) < cnt:
                    b.r[key] = cnt

    def op(self, eng, fn, reads=(), writes=()):
        self._wait(eng, self._deps(eng, reads, writes))
        inst = fn(eng.h)
        eng.cnt += 1
        inst.then_inc(eng.sem, 1)
        self._mark(eng.key, eng.cnt, reads, writes)
        return inst

    def pe_group(self, items, reads=(), writes=()):
        eng = self.pe
        self._wait(eng, self._deps(eng, reads, writes))
        inst = None
        for it in items:
            if it[0] == "mm":
                inst = eng.h.matmul(it[1], it[2], it[3], start=it[4], stop=it[5])
            else:
                inst = eng.h.transpose(it[1], it[2], it[3])
        eng.cnt += 1
        inst.then_inc(eng.sem, 1)
        self._mark(eng.key, eng.cnt, reads, writes)

    def dma(self, q, out_ap, in_ap, ds, reads=(), writes=()):
        ds = ds.for_queue(q)
        deps = self._deps(q, reads, writes)
        deps.pop(ds.key, None)
        self._wait(q, deps)
        inst = q.h.dma_start(out=out_ap, in_=in_ap)
        ds.cnt += 16
        inst.then_inc(ds.sem, 16)
        self._mark(ds.key, ds.cnt, reads, writes)

    def barrier(self):
        allv = {e.key: e.cnt for e in self.engs if e.cnt}
        for d in self.dsubs:
            if d.cnt:
                allv[d.key] = d.cnt
        for e in self.engs:
            self._wait(e, {k: v for k, v in allv.items() if k != e.key})


class _Stop(Exception):
    pass


def build(cfg, use_cc=True, pair_groups=None, stop=None):
    c = cfg
    D, T, HG, HR, DFF, L = c.D, c.T, c.HG, c.HR, c.DFF, c.L
    KC, NT, KM, NB, NKD = c.KC, c.NT, c.KM, c.NB, c.NKD
    DMIX, VG, VR, QKG = c.DMIX, c.VG, c.VR, c.QKG
    nc = bass.Bass("TRN2", target_bir_lowering=False)
    K = Ctx(nc)
    pe, act, dve, pool, sp = K.pe, K.act, K.dve, K.pool, K.sp

    def din(name, shape, dt=F32):
        return nc.dram_tensor(name, list(shape), dt, kind="ExternalInput").ap()

    x_in = din("x", [T, D])
    pos_in = din("pos", [128, T], I32)
    w_in = din("w_in", [L, D, c.DIN])
    gup_in = din("gup", [L, 17, QKG])
    an_in = din("an", [L, 128, D])
    mn_in = din("mn", [L, 128, D])
    fn_in = din("fn", [128, D])
    gon_in = din("gon", [L, 128, VG])
    rng_in = din("rng", [L, 128, VR])
    rnb_in = din("rnb", [L, 128, VR])
    w_out = din("w_out", [L, DMIX, D])
    w_up = din("w_up", [L, D, DFF])
    w_down = din("w_down", [L, DFF, D])
    ident_in = din("ident", [128, 128])
    tri_in = din("tri", [128, 128])
    mask_in = din("mask", [128, 128])
    rdec_in = din("rdec", [128, HR * 2 * 128])
    misc_in = din("misc", [128, 4])
    out = nc.dram_tensor("out", [T, D], F32, kind="ExternalOutput").ap()

    xs = nc.dram_tensor("xs", [T, D], F32).ap()
    qk_s = nc.dram_tensor("qk_s", [128, NB, T], BF16).ap()
    kd_s = nc.dram_tensor("kd_s", [T, NKD * 128], BF16).ap()
    v_s = nc.dram_tensor("v_s", [T, DMIX], BF16).ap()
    sg_s = nc.dram_tensor("sg_s", [T, DMIX], F32).ap()
    cs_s = nc.dram_tensor("cs_s", [2, 128, T], F32).ap()
    cc_in_t = [nc.dram_tensor("cc_in%d" % l, [NKD * 128, 256], F32) for l in range(L)]
    cc_out_t = [nc.dram_tensor("cc_out%d" % l, [2 * NKD * 128, 256], F32) for l in range(L)]

    dbufs = {}

    def DB(*key):
        b = dbufs.get(key)
        if b is None:
            b = dbufs[key] = Buf()
        return b

    banks = [nc.alloc_psum_tensor("bank%d" % i, [128, 512], F32) for i in range(8)]
    bankb = [Buf(excl=True) for _ in range(8)]

    def bf(ap):
        return ap.bitcast(BF16)

    es0 = contextlib.ExitStack()

    sbn = [0]

    def sb(es, name, shape, dt):
        sbn[0] += 1
        return es.enter_context(nc.sbuf_tensor("%s_s%d" % (name, sbn[0]), list(shape), dt))

    ident_b = sb(es0, "ident_b", [128, 128], BF16)
    tri_f = sb(es0, "tri_f", [128, 128], F32)
    mask_f = sb(es0, "mask_f", [128, 128], F32)
    misc = sb(es0, "misc", [128, 4], F32)
    d_all = sb(es0, "d_all", [128, HG * NT], F32)
    stat = sb(es0, "stat", [128, 64], F32)
    cB = Buf()
    dB, S1B, SbB = Buf(), [Buf() for _ in range(NKD)], [Buf() for _ in range(NKD)]
    cs = K.dsem("const")
    K.dma(pool, ident_b[:], ident_in, cs, writes=[cB])
    K.dma(sp, tri_f[:], tri_in, cs, writes=[cB])
    K.dma(sp, mask_f[:], mask_in, cs, writes=[cB])
    K.dma(sp, misc[:], misc_in, cs, writes=[cB])

    gammas = {0.0: 1.0}
    hgam = [1.0 - 2.0 ** (-5.0 - h) for h in range(HR)]

    with contextlib.ExitStack() as es:
        cosT = sb(es, "cosT", [128, T], F32)
        sinT = sb(es, "sinT", [128, T], F32)
        posi = sb(es, "posi", [128, T], I32)
        ang = sb(es, "ang", [128, T], F32)
        t1 = sb(es, "rt1", [128, T], F32)
        t2 = sb(es, "rt2", [128, T], F32)
        pB, aB, t1B, t2B, cosB, sinB = Buf(), Buf(), Buf(), Buf(), Buf(), Buf()
        ds = K.dsem("pos")
        K.dma(sp, posi[:], pos_in, ds, writes=[pB])
        K.op(dve, lambda e: e.tensor_copy(t1[:], posi[:]), [pB], [t1B])
        K.op(dve, lambda e: e.tensor_scalar(ang[:], t1[:], misc[:, 0:1], None, op0=ALU.mult), [t1B, cB], [aB])
        for which, dst, dstB in (("sin", sinT, sinB), ("cos", cosT, cosB)):
            off = 0.0 if which == "sin" else 0.25
            K.op(dve, lambda e: e.tensor_scalar(t1[:], ang[:], 1.0 / TWO_PI, off, op0=ALU.mult, op1=ALU.add), [aB], [t1B])
            K.op(dve, lambda e: e.tensor_scalar(t2[:], t1[:], RND, None, op0=ALU.add), [t1B], [t2B])
            K.op(dve, lambda e: e.tensor_scalar(t2[:], t2[:], -RND, None, op0=ALU.add), [t2B], [t2B])
            K.op(dve, lambda e: e.scalar_tensor_tensor(out=t1[:], in0=t2[:], scalar=-CW1, in1=ang[:], op0=ALU.mult, op1=ALU.add), [t2B, aB], [t1B])
            K.op(dve, lambda e: e.scalar_tensor_tensor(out=t1[:], in0=t2[:], scalar=-CW2, in1=t1[:], op0=ALU.mult, op1=ALU.add), [t2B, t1B], [t1B])
            sh = 0.0 if which == "sin" else float(np.pi / 2)
            K.op(dve, lambda e: e.tensor_scalar(t2[:], t1[:], sh, 3.1415925, op0=ALU.add, op1=ALU.min), [t1B], [t2B])
            K.op(dve, lambda e: e.tensor_scalar(t1[:], t2[:], -3.1415925, None, op0=ALU.max), [t2B], [t1B])
            K.op(act, lambda e: e.activation(out=dst[:], in_=t1[:], func=AF.Sin), [t1B], [dstB])
        K.dma(sp, cs_s[0], cosT[:], K.dsem("cso"), reads=[cosB], writes=[DB("cs", 0)])
        K.dma(sp, cs_s[1], sinT[:], K.dsem("cso"), reads=[sinB], writes=[DB("cs", 1)])
        K.barrier()

    def rstd_from_ms(col_ms, col_out, rB):
        K.op(act, lambda e: e.activation(out=stat[:, col_out:col_out + 1], in_=stat[:, col_ms:col_ms + 1],
                                         func=AF.Ln, bias=EPS, scale=1.0), [rB], [rB])
        K.op(act, lambda e: e.activation(out=stat[:, col_out:col_out + 1], in_=stat[:, col_out:col_out + 1],
                                         func=AF.Exp, scale=-0.5), [rB], [rB])

    nstB = [Buf(), Buf()]

    def norm_to_T(xt, xB, gam, gamB, hT, hTB, tok0, h_bfs, hBs, junk, par, pbanks):
        c0 = 20 + 2 * par
        stB = nstB[par]
        h_bf, hB = h_bfs[par], hBs[par]
        K.op(act, lambda e: e.activation(out=junk[:], in_=xt, func=AF.Square, scale=float(D ** -0.5),
                                         accum_out=stat[:, c0:c0 + 1]), [xB], [stB])
        rstd_from_ms(c0, c0 + 1, stB)
        K.op(dve, lambda e: e.scalar_tensor_tensor(out=h_bf[:], in0=xt, scalar=stat[:, c0 + 1:c0 + 2], in1=gam,
                                                   op0=ALU.mult, op1=ALU.mult), [xB, stB, gamB], [hB])
        for gi, k0 in enumerate(range(0, KC, 4)):
            nk = min(4, KC - k0)
            pbk = pbanks[gi % len(pbanks)]
            pb = bf(banks[pbk][:])
            K.pe_group([("tr", pb[:, j * 128:(j + 1) * 128], h_bf[:, (k0 + j) * 128:(k0 + j + 1) * 128], ident_b[:])
                        for j in range(nk)], [hB, cB], [bankb[pbk]])
            src = pb[:, 0:nk * 128].rearrange("p (k t) -> p k t", k=nk)
            dst = hT[:, k0:k0 + nk, tok0:tok0 + 128]
            if gi % 2 == 0:
                K.op(act, lambda e: e.copy(dst, src), [bankb[pbk]], [hTB])
            else:
                K.op(dve, lambda e: e.tensor_copy(dst, src), [bankb[pbk]], [hTB])

    def chk(name):
        if stop == name:
            raise _Stop()

    def _layers():
        chk('rot')
        for l in range(L):
            K.new_layer()
            x_src = x_in if l == 0 else xs
            last = l == L - 1

            with contextlib.ExitStack() as es:
                hT = sb(es, "hT", [128, KC, T], BF16)
                hTB = [Buf() for _ in range(NT)]
                sp_all = sb(es, "sp_all", [128, NT, QKG], F32)
                spB = [Buf() for _ in range(NT)]
                stB = Buf()
                S1 = sb(es, "S1", [128, NKD, 256], F32)
                rdec = sb(es, "rdec", [128, HR * 2 * 128], F32)
                rotB = Buf()
                K.dma(sp, rdec[:], rdec_in, K.dsem("rdec"), writes=[rotB])
                K.op(dve, lambda e: e.memset(S1[:], 0.0), [], S1B)
                NWS = 3
                wring = [sb(es, "wA%d" % i, [128, KC, 512], BF16) for i in range(NWS)]
                wrB = [Buf() for _ in range(NWS)]
                wrds = [K.dsem("wA") for _ in range(NWS)]

                heads = [("g", h, c.o_gq + h * 128, c.o_gk + h * 128, c.o_gv + h * 256, c.o_gg + h * 256, 1) for h in range(HG)]
                heads += [("r", h, c.o_rq + h * 256, c.o_rk + h * 256, c.o_rv + h * 256, c.o_rg + h * 256, 2) for h in range(HR)]

                def load_set(i):
                    if i >= 2 * len(heads):
                        return
                    kind, h, cq, ck_, cv, cg, nblk = heads[i // 2]
                    s = i % NWS
                    if i % 2 == 0:
                        parts = ((cq, 128 * nblk, 0), (ck_, 128 * nblk, 256))
                    else:
                        parts = ((cv, 256, 0), (cg, 256, 256))
                    for (c0, n, dst) in parts:
                        K.dma(pool, wring[s][:, :, dst:dst + n],
                              w_in[l][:, c0:c0 + n].rearrange("(k p) c -> p k c", p=128), wrds[s], writes=[wrB[s]])

                load_set(0)
                load_set(1)

                with contextlib.ExitStack() as es2:
                    gam = sb(es2, "gamA", [128, D], F32)
                    gamB = Buf()
                    xt2 = [sb(es2, "xtA%d" % i, [128, D], F32) for i in range(2)]
                    xtB = [Buf(), Buf()]
                    xds = [K.dsem("xA"), K.dsem("xA")]
                    h_bf = [sb(es2, "h_bfA%d" % i, [128, D], BF16) for i in range(2)]
                    hB = [Buf(), Buf()]
                    junk = sb(es2, "junkA", [128, D], BF16)
                    K.dma(sp, gam[:], an_in[l], K.dsem("gamA"), writes=[gamB])
                    K.dma(sp, xt2[0][:], x_src[0:128, :], xds[0], writes=[xtB[0]])
                    for tt in range(NT):
                        s = tt % 2
                        if tt + 1 < NT:
                            K.dma(sp, xt2[1 - s][:], x_src[(tt + 1) * 128:(tt + 2) * 128, :], xds[1 - s], writes=[xtB[1 - s]])
                        norm_to_T(xt2[s][:], xtB[s], gam[:], gamB, hT, hTB[tt], tt * 128, h_bf, hB, junk, s, [0, 1])
                    K.barrier()
                chk('A0')

                with contextlib.ExitStack() as es2:
                    grT = sb(es2, "grT", [32, T], F32)
                    grB = Buf()
                    gup = sb(es2, "gup", [32, QKG], F32)
                    gupB = Buf()
                    wgr = sb(es2, "wgr", [128, KC, 16], BF16)
                    wgrB = Buf()
                    ez = sb(es2, "ez", [128, QKG], F32)
                    ezB = Buf()
                    K.op(dve, lambda e: e.memset(grT[:], 1.0), [], [grB])
                    K.dma(sp, gup[0:17, :], gup_in[l], K.dsem("gupA"), writes=[gupB])
                    K.dma(pool, wgr[:], w_in[l][:, c.o_gr:c.o_gr + 16].rearrange("(k p) c -> p k c", p=128), K.dsem("wgrA"), writes=[wgrB])
                    for tg in range(0, T, 512):
                        n = min(512, T - tg)
                        tiles = list(range(tg // 128, (tg + n) // 128))
                        K.pe_group([("mm", banks[1][0:16, 0:n], wgr[:, kc, :], hT[:, kc, tg:tg + n], kc == 0, kc == KC - 1)
                                    for kc in range(KC)], [wgrB] + [hTB[t] for t in tiles], [bankb[1]])
                        K.op(act, lambda e: e.copy(grT[0:16, tg:tg + n], banks[1][0:16, 0:n]), [bankb[1]], [grB])
                    for tt in range(NT):
                        K.pe_group([("mm", banks[2][:, 0:QKG], grT[0:17, tt * 128:(tt + 1) * 128], gup[0:17, :], True, True)],
                                   [grB, gupB], [bankb[2]])
                        K.op(act, lambda e: e.activation(out=ez[:], in_=banks[2][:, 0:QKG], func=AF.Exp, scale=-1.0), [bankb[2]], [ezB])
                        K.op(act, lambda e: e.activation(out=sp_all[:, tt, :], in_=ez[:], func=AF.Ln, bias=1.0, scale=1.0), [ezB], [spB[tt]])
                    K.barrier()
                chk('A1')

                cosT = sb(es, "cosT", [128, T], F32)
                sinT = sb(es, "sinT", [128, T], F32)
                K.dma(sp, cosT[:], cs_s[0], K.dsem("cos"), reads=[DB("cs", 0)], writes=[rotB])
                K.dma(sp, sinT[:], cs_s[1], K.dsem("sin"), reads=[DB("cs", 1)], writes=[rotB])
                eb = sb(es, "eb", [128, 512], F32)
                enb = sb(es, "enb", [128, 512], F32)
                ebB, enbB = Buf(), Buf()
                qf = [sb(es, "qf%d" % i, [128, 512], F32) for i in range(2)]
                qfB = [Buf(), Buf()]
                rt = [sb(es, "rtA%d" % i, [128, 512], F32) for i in range(4)]
                rtB = [Buf() for _ in range(4)]
                fm = [sb(es, "fm%d" % i, [128, 512], BF16) for i in range(4)]
                fmB = [Buf() for _ in range(4)]
                fmds = [K.dsem("fm") for _ in range(4)]
                kdt = [sb(es, "kdt%d" % i, [128, 256], BF16) for i in range(4)]
                kdtB = [Buf() for _ in range(4)]
                kdds = [K.dsem("kd") for _ in range(4)]
                vt = [sb(es, "vt%d" % i, [128, 256], BF16) for i in range(4)]
                vtB = [Buf() for _ in range(4)]
                vds = [K.dsem("v") for _ in range(4)]
                sgt = [sb(es, "sgt%d" % i, [128, 256], F32) for i in range(4)]
                sgtB = [Buf() for _ in range(4)]
                sgds = [K.dsem("sg") for _ in range(4)]

                fmi = 0
                tok_i = [0]
                for hi, (kind, h, cq, ck, cv, cg, nblk) in enumerate(heads):
                    if hi == 1:
                        chk('A2f')
                    load_set(2 * hi + 2)
                    wt, wtB = wring[(2 * hi) % NWS], wrB[(2 * hi) % NWS]
                    wv, wvB = wring[(2 * hi + 1) % NWS], wrB[(2 * hi + 1) % NWS]
                    sblk0 = h if kind == "g" else HG + 2 * h
                    qblk0 = 2 * h if kind == "g" else 2 * HG + 4 * h
                    for tg in range(0, T, 512):
                        n = min(512, T - tg)
                        tiles = list(range(tg // 128, (tg + n) // 128))
                        hr = [hTB[t] for t in tiles]
                        if kind == "g":
                            K.pe_group([("mm", banks[1][:, j * 128:(j + 1) * 128], sp_all[:, t, h * 128:(h + 1) * 128], tri_f[:], True, True)
                                        for j, t in enumerate(tiles)], [spB[t] for t in tiles] + [cB], [bankb[1]])
                            K.op(act, lambda e: e.activation(out=eb[:, 0:n], in_=banks[1][:, 0:n], func=AF.Exp), [bankb[1]], [ebB])
                            K.op(act, lambda e: e.activation(out=enb[:, 0:n], in_=banks[1][:, 0:n], func=AF.Exp, scale=-1.0), [bankb[1]], [enbB])
                            for j, t in enumerate(tiles):
                                K.op(dve, lambda e: e.tensor_copy(d_all[:, h * NT + t:h * NT + t + 1], eb[:, j * 128 + 127:j * 128 + 128]), [ebB], [dB])
                        chk('A2a')
                        for qk in range(2):
                            wcol = 0 if qk == 0 else 256
                            for blk in range(nblk):
                                bk = 2 + (qk * 2 + blk) % 2 if kind == "g" else 2 + blk
                                pbk = banks[bk]
                                K.pe_group([("mm", pbk[:, 0:n], wt[:, kc, wcol + blk * 128:wcol + (blk + 1) * 128], hT[:, kc, tg:tg + n], kc == 0, kc == KC - 1)
                                            for kc in range(KC)], [wtB] + hr, [bankb[bk]])
                                if kind == "g":
                                    f = fmi % 4
                                    fmi += 1
                                    if qk == 0:
                                        K.op(dve, lambda e: e.scalar_tensor_tensor(out=fm[f][:, 0:n], in0=pbk[:, 0:n], scalar=float(128 ** -0.5), in1=eb[:, 0:n],
                                                                                   op0=ALU.mult, op1=ALU.mult), [bankb[bk], ebB], [fmB[f]])
                                    else:
                                        K.op(dve, lambda e: e.tensor_tensor(out=fm[f][:, 0:n], in0=pbk[:, 0:n], in1=enb[:, 0:n], op=ALU.mult), [bankb[bk], enbB], [fmB[f]])
                                    K.dma(sp, qk_s[:, qblk0 + qk, tg:tg + n], fm[f][:, 0:n], fmds[f], reads=[fmB[f]], writes=[DB("qk", qblk0 + qk, tg)])
                                    if qk == 1:
                                        kf = [f]
                                else:
                                    dec = rdec[:, (h * 2 + qk) * 128:(h * 2 + qk + 1) * 128]
                                    for j in range(n // 128):
                                        K.op(dve, lambda e: e.tensor_tensor(out=qf[blk][:, j * 128:(j + 1) * 128], in0=pbk[:, j * 128:(j + 1) * 128], in1=dec, op=ALU.mult),
                                             [bankb[bk], rotB], [qfB[blk]])
                            if kind == "r":
                                fs = []
                                for blk in range(2):
                                    a, b_ = (qf[0], qf[1]) if blk == 0 else (qf[1], qf[0])
                                    aB_, bB_ = (qfB[0], qfB[1]) if blk == 0 else (qfB[1], qfB[0])
                                    r0, r1 = rt[2 * blk], rt[2 * blk + 1]
                                    r0B, r1B = rtB[2 * blk], rtB[2 * blk + 1]
                                    K.op(pool, lambda e: e.tensor_tensor(out=r0[:, 0:n], in0=a[:, 0:n], in1=cosT[:, tg:tg + n], op=ALU.mult), [aB_, rotB], [r0B])
                                    K.op(pool, lambda e: e.tensor_tensor(out=r1[:, 0:n], in0=b_[:, 0:n], in1=sinT[:, tg:tg + n], op=ALU.mult), [bB_, rotB], [r1B])
                                    f = fmi % 4
                                    fmi += 1
                                    K.op(dve, lambda e: e.tensor_tensor(out=fm[f][:, 0:n], in0=r0[:, 0:n], in1=r1[:, 0:n],
                                                                        op=(ALU.subtract if blk == 0 else ALU.add)), [r0B, r1B], [fmB[f]])
                                    bi = qblk0 + qk * 2 + blk
                                    K.dma(sp, qk_s[:, bi, tg:tg + n], fm[f][:, 0:n], fmds[f], reads=[fmB[f]], writes=[DB("qk", bi, tg)])
                                    fs.append(f)
                                if qk == 1:
                                    kf = fs
                        if tg + 512 >= T:
                            load_set(2 * hi + 3)
                        chk('A2b')
                        vcol = (h * 256) if kind == "g" else (VG + h * 256)
                        slot = {}

                        def e_vg(j, t):
                            si = tok_i[0] % 4
                            bk = (5, 6, 0)[tok_i[0] % 3]
                            tok_i[0] += 1
                            slot[j] = si
                            K.pe_group([("mm", banks[bk][:, :], hT[:, kc, t * 128:(t + 1) * 128], wv[:, kc, 0:512], kc == 0, kc == KC - 1) for kc in range(KC)],
                                       [wvB, hTB[t]], [bankb[bk]])
                            K.op(dve, lambda e: e.tensor_copy(vt[si][:], banks[bk][:, 0:256]), [bankb[bk]], [vtB[si]])
                            K.op(act, lambda e: e.activation(out=sgt[si][:], in_=banks[bk][:, 256:512], func=AF.Silu), [bankb[bk]], [sgtB[si]])
                            K.dma(sp, v_s[t * 128:(t + 1) * 128, vcol:vcol + 256], vt[si][:], vds[si], reads=[vtB[si]], writes=[DB("v", vcol, t)])
                            K.dma(sp, sg_s[t * 128:(t + 1) * 128, vcol:vcol + 256], sgt[si][:], sgds[si], reads=[sgtB[si]], writes=[DB("sg", vcol, t)])

                        def e_trl(j, t):
                            si = slot[j]
                            pb = bf(banks[4][:])
                            K.pe_group([("tr", pb[:, b2 * 128:(b2 + 1) * 128], fm[kf[b2]][:, j * 128:(j + 1) * 128], ident_b[:]) for b2 in range(nblk)],
                                       [fmB[x] for x in kf] + [cB], [bankb[4]])
                            K.op(act, lambda e: e.copy(kdt[si][:, 0:nblk * 128], pb[:, 0:nblk * 128]), [bankb[4]], [kdtB[si]])
                            K.dma(sp, kd_s[t * 128:(t + 1) * 128, sblk0 * 128:(sblk0 + nblk) * 128], kdt[si][:, 0:nblk * 128], kdds[si],
                                  reads=[kdtB[si]], writes=[DB("kd", sblk0, t)])

                        def e_local(j, t):
                            si = slot[j]
                            lb = 1 if (kind == "r" and j % 2 == 1) else 7
                            for b2 in range(nblk):
                                K.pe_group([("mm", banks[lb][:, b2 * 256:(b2 + 1) * 256], kdt[si][:, b2 * 128:(b2 + 1) * 128], vt[si][:], True, True)],
                                           [kdtB[si], vtB[si]], [bankb[lb]])
                            for b2 in range(nblk):
                                sbk = sblk0 + b2
                                dsc = d_all[:, h * NT + t:h * NT + t + 1] if kind == "g" else float(hgam[h] ** 128)
                                K.op(dve, lambda e: e.tensor_scalar(S1[:, sbk, :], S1[:, sbk, :], dsc, None, op0=ALU.mult), [S1B[sbk], dB], [S1B[sbk]])
                                K.op(dve, lambda e: e.scalar_tensor_tensor(out=S1[:, sbk, :], in0=banks[lb][:, b2 * 256:(b2 + 1) * 256], scalar=dsc, in1=S1[:, sbk, :],
                                                                           op0=ALU.mult, op1=ALU.add), [bankb[lb], S1B[sbk], dB], [S1B[sbk]])

                        AHEAD = 3
                        ntl = len(tiles)
                        for j in range(min(AHEAD, ntl)):
                            e_vg(j, tiles[j])
                        for j in range(ntl):
                            e_trl(j, tiles[j])
                            if j + AHEAD < ntl:
                                e_vg(j + AHEAD, tiles[j + AHEAD])
                            e_local(j, tiles[j])
                chk('A2')
                ccds = K.dsem("cc")
                ccB, ccoB = Buf(), Buf()
                K.dma(sp, cc_in_t[l].ap().rearrange("(b p) v -> p b v", p=128), S1[:], ccds, reads=S1B, writes=[ccB])
                K.barrier()

            chk('A')
            with contextlib.ExitStack() as es:
                S1 = sb(es, "S1b", [128, NKD, 256], F32)
                Sb = sb(es, "Sb", [128, NKD, 256], BF16)
                wo = sb(es, "wo", [128, KM, D], BF16)
                woB = Buf()
                gon = sb(es, "gon", [128, VG], F32)
                rgn = sb(es, "rgn", [128, VR], F32)
                rbn = sb(es, "rbn", [128, VR], F32)
                nB = Buf()
                qk2 = [sb(es, "qkB%d" % i, [128, NB, 128], BF16) for i in range(2)]
                kd2 = [sb(es, "kdB%d" % i, [128, NKD * 128], BF16) for i in range(2)]
                v2 = [sb(es, "vB%d" % i, [128, DMIX], BF16) for i in range(2)]
                sg2 = [sb(es, "sgB%d" % i, [128, DMIX], F32) for i in range(2)]
                x2 = [sb(es, "xB%d" % i, [128, D], F32) for i in range(2)]
                inB = [Buf(), Buf()]
                xB = [Buf(), Buf()]
                inds = [K.dsem("inB"), K.dsem("inB")]
                xds = [K.dsem("xB"), K.dsem("xB")]
                xods = [K.dsem("xoB"), K.dsem("xoB")]
                sc = [sb(es, "sc%d" % i, [128, 128], BF16) for i in range(2)]
                scB = [Buf(), Buf()]
                tn = [sb(es, "tn%d" % i, [128, 256], F32) for i in range(2)]
                tnB = [Buf(), Buf()]
                junk = sb(es, "junkB", [128, 256], BF16)
                mix = [sb(es, "mix%d" % i, [128, DMIX], BF16) for i in range(2)]
                mixB = [Buf(), Buf()]
                mixT = sb(es, "mixT", [128, KM, 128], BF16)
                mixTB = Buf()
                stB = Buf()

                cds = K.dsem("cB")
                for g4 in range(0, D, 512):
                    n4 = min(512, D - g4)
                    K.dma(pool, wo[:, :, g4:g4 + n4], w_out[l][:, g4:g4 + n4].rearrange("(k p) c -> p k c", p=128), cds, writes=[woB])
                K.dma(sp, gon[:], gon_in[l], cds, writes=[nB])
                K.dma(sp, rgn[:], rng_in[l], cds, writes=[nB])
                K.dma(sp, rbn[:], rnb_in[l], cds, writes=[nB])
                if use_cc:
                    ccsem = nc.alloc_semaphore("ccs%d" % l)
                    nc.gpsimd.collective_compute("AllGather", ALU.bypass, replica_groups=pair_groups,
                                                 ins=[cc_in_t[l].ap().opt()], outs=[cc_out_t[l].ap().opt()]).then_inc(ccsem)
                    nc.gpsimd.wait_ge(ccsem, 1)
                    src = cc_out_t[l].ap()[0:NKD * 128, :]
                else:
                    src = cc_in_t[l].ap()
                K.dma(pool, S1[:], src.rearrange("(b p) v -> p b v", p=128), K.dsem("ccl"), reads=[ccB], writes=S1B)
                K.op(dve, lambda e: e.tensor_scalar(S1[:], S1[:], misc[:, 1:2], None, op0=ALU.mult), S1B + [cB], S1B)
                K.op(dve, lambda e: e.tensor_copy(Sb[:], S1[:]), S1B, SbB)

                def load_tile(t):
                    s = t % 2
                    tk = slice(t * 128, (t + 1) * 128)
                    K.dma(sp, qk2[s][:], qk_s[:, :, tk], inds[s], reads=[DB("qk", b_, (t // 4) * 512) for b_ in range(NB)], writes=[inB[s]])
                    K.dma(sp, kd2[s][:], kd_s[tk, :], inds[s], reads=[DB("kd", b_, t) for b_ in range(NKD)], writes=[inB[s]])
                    K.dma(sp, v2[s][:], v_s[tk, :], inds[s], reads=[DB("v", cc_, t) for cc_ in range(0, DMIX, 256)], writes=[inB[s]])
                    K.dma(sp, sg2[s][:], sg_s[tk, :], inds[s], reads=[DB("sg", cc_, t) for cc_ in range(0, DMIX, 256)], writes=[inB[s]])

                def load_x(t):
                    s = t % 2
                    tk = slice(t * 128, (t + 1) * 128)
                    K.dma(sp, x2[s][:], x_src[tk, :], xds[s], reads=[DB("xs", t)] if l > 0 else [], writes=[xB[s]])

                hi_all = [("g", h) for h in range(HG)] + [("r", h) for h in range(HR)]
                NHh = len(hi_all)

                def hinfo(idx):
                    kind, h = hi_all[idx]
                    nblk = 1 if kind == "g" else 2
                    sblk0 = h if kind == "g" else HG + 2 * h
                    qb0 = 2 * h if kind == "g" else 2 * HG + 4 * h
                    vcol = (h * 256) if kind == "g" else (VG + h * 256)
                    return kind, h, nblk, sblk0, qb0, vcol, idx % 2

                def e_scores(t, idx):
                    kind, h, nblk, sblk0, qb0, vcol, i2 = hinfo(idx)
                    s = t % 2
                    K.pe_group([("mm", banks[i2][:, 0:128], qk2[s][:, qb0 + nblk + b2, :], qk2[s][:, qb0 + b2, :], b2 == 0, b2 == nblk - 1) for b2 in range(nblk)],
                               [inB[s]], [bankb[i2]])
                    K.op(dve, lambda e: e.tensor_tensor(out=sc[i2][:], in0=banks[i2][:, 0:128], in1=mask_f[:], op=ALU.mult), [bankb[i2], cB], [scB[i2]])

                def e_head(t, idx):
                    kind, h, nblk, sblk0, qb0, vcol, i2 = hinfo(idx)
                    s = t % 2
                    pbo, pbl = 2 + i2, 4 + i2
                    items = [("mm", banks[pbo][:, 0:256], sc[i2][:], v2[s][:, vcol:vcol + 256], True, False)]
                    for b2 in range(nblk):
                        items.append(("mm", banks[pbo][:, 0:256], qk2[s][:, qb0 + b2, :], Sb[:, sblk0 + b2, :], False, b2 == nblk - 1))
                    K.pe_group(items, [scB[i2], inB[s]] + [SbB[sblk0 + b2] for b2 in range(nblk)], [bankb[pbo]])
                    for b2 in range(nblk):
                        K.pe_group([("mm", banks[pbl][:, b2 * 256:(b2 + 1) * 256], kd2[s][:, (sblk0 + b2) * 128:(sblk0 + b2 + 1) * 128], v2[s][:, vcol:vcol + 256], True, True)],
                                   [inB[s]], [bankb[pbl]])
                    for b2 in range(nblk):
                        sbk = sblk0 + b2
                        if kind == "g":
                            dsc = d_all[:, h * NT + t:h * NT + t + 1]
                        else:
                            dsc = float(hgam[h] ** 128)
                        K.op(act, lambda e: e.activation(out=S1[:, sbk, :], in_=S1[:, sbk, :], func=AF.Identity, scale=dsc), [S1B[sbk], dB], [S1B[sbk]])
                        K.op(dve, lambda e: e.scalar_tensor_tensor(out=S1[:, sbk, :], in0=banks[pbl][:, b2 * 256:(b2 + 1) * 256], scalar=dsc, in1=S1[:, sbk, :],
                                                                   op0=ALU.mult, op1=ALU.add), [bankb[pbl], S1B[sbk], dB], [S1B[sbk]])
                        K.op(pool, lambda e: e.tensor_copy(Sb[:, sbk, :], S1[:, sbk, :]), [S1B[sbk]], [SbB[sbk]])
                    po = banks[pbo][:, 0:256]
                    c0 = 2 + 6 * i2
                    if kind == "g":
                        K.op(act, lambda e: e.activation(out=junk[:], in_=po, func=AF.Square, scale=float(256 ** -0.5), accum_out=stat[:, c0:c0 + 1]), [bankb[pbo]], [stB])
                        rstd_from_ms(c0, c0 + 1, stB)
                        K.op(dve, lambda e: e.scalar_tensor_tensor(out=tn[i2][:], in0=po, scalar=stat[:, c0 + 1:c0 + 2], in1=gon[:, h * 256:(h + 1) * 256],
                                                                   op0=ALU.mult, op1=ALU.mult), [bankb[pbo], stB, nB], [tnB[i2]])
                    else:
                        K.op(act, lambda e: e.activation(out=junk[:], in_=po, func=AF.Identity, scale=float(1.0 / 256), accum_out=stat[:, c0:c0 + 1]), [bankb[pbo]], [stB])
                        K.op(act, lambda e: e.activation(out=junk[:], in_=po, func=AF.Square, scale=float(256 ** -0.5), accum_out=stat[:, c0 + 1:c0 + 2]), [bankb[pbo]], [stB])
                        K.op(dve, lambda e: e.tensor_tensor(out=stat[:, c0 + 2:c0 + 3], in0=stat[:, c0:c0 + 1], in1=stat[:, c0:c0 + 1], op=ALU.mult), [stB], [stB])
                        K.op(dve, lambda e: e.tensor_tensor(out=stat[:, c0 + 3:c0 + 4], in0=stat[:, c0 + 1:c0 + 2], in1=stat[:, c0 + 2:c0 + 3], op=ALU.subtract), [stB], [stB])
                        rstd_from_ms(c0 + 3, c0 + 4, stB)
                        K.op(dve, lambda e: e.scalar_tensor_tensor(out=tn[i2][:], in0=po, scalar=stat[:, c0:c0 + 1], in1=rgn[:, h * 256:(h + 1) * 256],
                                                                   op0=ALU.subtract, op1=ALU.mult), [bankb[pbo], stB, nB], [tnB[i2]])
                        K.op(dve, lambda e: e.scalar_tensor_tensor(out=tn[i2][:], in0=tn[i2][:], scalar=stat[:, c0 + 4:c0 + 5], in1=rbn[:, h * 256:(h + 1) * 256],
                                                                   op0=ALU.mult, op1=ALU.add), [tnB[i2], stB, nB], [tnB[i2]])
                    K.op(pool, lambda e: e.tensor_tensor(out=mix[s][:, vcol:vcol + 256], in0=tn[i2][:], in1=sg2[s][:, vcol:vcol + 256], op=ALU.mult), [tnB[i2], inB[s]], [mixB[s]])

                def o_pieces(t):
                    s = t % 2
                    pl = []
                    for k0 in range(0, KM, 4):
                        def tr_piece(k0=k0):
                            nk = min(4, KM - k0)
                            pb = bf(banks[6][:])
                            K.pe_group([("tr", pb[:, j * 128:(j + 1) * 128], mix[s][:, (k0 + j) * 128:(k0 + j + 1) * 128], ident_b[:]) for j in range(nk)], [mixB[s], cB], [bankb[6]])
                            K.op(act, lambda e: e.copy(mixT[:, k0:k0 + nk, :], pb[:, 0:nk * 128].rearrange("p (k t) -> p k t", k=nk)), [bankb[6]], [mixTB])
                        pl.append(tr_piece)
                    ng = len(range(0, D, 512))
                    for gi, g4 in enumerate(range(0, D, 512)):
                        def op_piece(gi=gi, g4=g4):
                            n = min(512, D - g4)
                            K.pe_group([("mm", banks[7][:, 0:n], mixT[:, km, :], wo[:, km, g4:g4 + n], km == 0, km == KM - 1) for km in range(KM)], [mixTB, woB], [bankb[7]])
                            K.op(dve, lambda e: e.tensor_tensor(out=x2[s][:, g4:g4 + n], in0=banks[7][:, 0:n], in1=x2[s][:, g4:g4 + n], op=ALU.add), [bankb[7], xB[s]], [xB[s]])
                            if gi == ng - 1:
                                K.dma(sp, xs[t * 128:(t + 1) * 128, :], x2[s][:], xods[s], reads=[xB[s]], writes=[DB("xs", t)])
                        pl.append(op_piece)
                    return pl

                load_tile(0)
                for t in range(NT + 1):
                    if t + 1 < NT:
                        load_tile(t + 1)
                    if t < NT:
                        load_x(t)
                    pl = o_pieces(t - 1) if t >= 1 else []
                    npc = len(pl)
                    if t < NT:
                        e_scores(t, 0)
                        for idx in range(NHh):
                            if idx + 1 < NHh:
                                e_scores(t, idx + 1)
                            e_head(t, idx)
                            for pc in pl[idx * npc // NHh:(idx + 1) * npc // NHh]:
                                pc()
                    else:
                        for pc in pl:
                            pc()
                K.barrier()

            chk('B1')
            with contextlib.ExitStack() as es:
                TBF = min(c.TBF, T)
                NTB = TBF // 128
                FG = 512
                gam = sb(es, "gamF", [128, D], F32)
                gamB = Buf()
                xg = sb(es, "xg", [128, NTB, D], F32)
                xgB = [Buf() for _ in range(NTB)]
                xgds = [K.dsem("xg") for _ in range(NTB)]
                xods = [K.dsem("xgo") for _ in range(NTB)]
                h2T = sb(es, "h2T", [128, KC, TBF], BF16)
                h2TB = [Buf() for _ in range(NTB)]
                h_bf = [sb(es, "h_bfF%d" % i, [128, D], BF16) for i in range(2)]
                hB = [Buf(), Buf()]
                junk = sb(es, "junkF", [128, D], BF16)
                stB = Buf()
                wu = [sb(es, "wu%d" % i, [128, KC, FG], BF16) for i in range(2)]
                wuB = [Buf(), Buf()]
                wuds = [K.dsem("wu"), K.dsem("wu")]
                wd = [sb(es, "wd%d" % i, [128, FG // 128, D], BF16) for i in range(2)]
                wdB = [Buf(), Buf()]
                wdds = [K.dsem("wd"), K.dsem("wd")]
                uT = [sb(es, "uT%d" % i, [128, FG // 128, TBF], BF16) for i in range(2)]
                uTB = [Buf(), Buf()]
                sq = [sb(es, "sq%d" % i, [128, 512], F32) for i in range(2)]
                sqB = [Buf(), Buf()]
                gds = K.dsem("gF")
                NFG = DFF // FG
                nload = [0]

                def load_w(fi, bi=0):
                    s = (bi * NFG + fi) % 2
                    K.dma(pool, wu[s][:], w_up[l][:, fi * FG:(fi + 1) * FG].rearrange("(k p) c -> p k c", p=128), wuds[s], writes=[wuB[s]])
                    K.dma(pool, wd[s][:], w_down[l][fi * FG:(fi + 1) * FG, :].rearrange("(k p) c -> p k c", p=128), wdds[s], writes=[wdB[s]])

                def load_xg(tb_, j):
                    t_ = tb_ // 128 + j
                    K.dma(sp, xg[:, j, :], xs[t_ * 128:(t_ + 1) * 128, :], xgds[j], reads=[DB("xs", t_)], writes=[xgB[j]])

                nblk_f = T // TBF
                for bi, tb in enumerate(range(0, T, TBF)):
                    has_next = bi + 1 < nblk_f
                    if bi == 0:
                        load_w(0)
                        for j in range(NTB):
                            load_xg(tb, j)
                    if bi == 0 or last:
                        K.dma(sp, gam[:], mn_in[l], gds, writes=[gamB])
                    ev = 0
                    for fi in range(NFG):
                        s = (bi * NFG + fi) % 2
                        if fi + 1 < NFG:
                            load_w(fi + 1, bi)
                        elif has_next:
                            load_w(0, bi + 1)
                        for tg in range(0, TBF, 512):
                            n = min(512, TBF - tg)
                            if fi == 0:
                                for j in range(tg // 128, (tg + n) // 128):
                                    norm_to_T(xg[:, j, :], xgB[j], gam[:], gamB, h2T, h2TB[j], j * 128, h_bf, hB, junk, j % 2, [0, 7])
                            for fc in range(FG // 128):
                                e2 = ev % 2
                                ev += 1
                                bk = 1 + e2
                                K.pe_group([("mm", banks[bk][:, 0:n], wu[s][:, kc, fc * 128:(fc + 1) * 128], h2T[:, kc, tg:tg + n], kc == 0, kc == KC - 1) for kc in range(KC)],
                                           [wuB[s]] + h2TB[tg // 128:(tg + n) // 128], [bankb[bk]])
                                K.op(act, lambda e: e.activation(out=sq[e2][:, 0:n], in_=banks[bk][:, 0:n], func=AF.Square), [bankb[bk]], [sqB[e2]])
                                K.op(dve, lambda e: e.scalar_tensor_tensor(out=uT[s][:, fc, tg:tg + n], in0=banks[bk][:, 0:n], scalar=0.0, in1=sq[e2][:, 0:n],
                                                                           op0=ALU.is_gt, op1=ALU.mult), [bankb[bk], sqB[e2]], [uTB[s]])
                        di = 0
                        for j in range(NTB):
                            for g4 in range(0, D, 512):
                                n = min(512, D - g4)
                                bk = 3 + di % 4
                                di += 1
                                K.pe_group([("mm", banks[bk][:, 0:n], uT[s][:, fc, j * 128:(j + 1) * 128], wd[s][:, fc, g4:g4 + n], fc == 0, fc == FG // 128 - 1) for fc in range(FG // 128)],
                                           [uTB[s], wdB[s]], [bankb[bk]])
                                K.op(dve, lambda e: e.tensor_tensor(out=xg[:, j, g4:g4 + n], in0=banks[bk][:, 0:n], in1=xg[:, j, g4:g4 + n], op=ALU.add), [bankb[bk], xgB[j]], [xgB[j]])
                            if fi == NFG - 1 and not last:
                                t = tb // 128 + j
                                K.dma(sp, xs[t * 128:(t + 1) * 128, :], xg[:, j, :], xods[j], reads=[xgB[j]], writes=[DB("xs", t)])
                                if has_next:
                                    load_xg(tb + TBF, j)
                    if last:
                        K.dma(sp, gam[:], fn_in, gds, writes=[gamB])
                    for j in range(NTB):
                        t = tb // 128 + j
                        if not last:
                            pass
                        else:
                            K.op(act, lambda e: e.activation(out=junk[:], in_=xg[:, j, :], func=AF.Square, scale=float(D ** -0.5), accum_out=stat[:, 0:1]), [xgB[j]], [stB])
                            rstd_from_ms(0, 1, stB)
                            K.op(dve, lambda e: e.scalar_tensor_tensor(out=xg[:, j, :], in0=xg[:, j, :], scalar=stat[:, 1:2], in1=gam[:], op0=ALU.mult, op1=ALU.mult),
                                 [xgB[j], stB, gamB], [xgB[j]])
                            K.dma(sp, out[t * 128:(t + 1) * 128, :], xg[:, j, :], xods[j], reads=[xgB[j]], writes=[DB("out", t)])
                            if has_next:
                                load_xg(tb + TBF, j)
                K.barrier()

    try:
        _layers()
        K.barrier()
        es0.close()
    except _Stop:
        K.barrier()
    return nc


def make_consts(cfg):
    HR = cfg.HR
    ident = np.eye(128, dtype=np.float32)
    j = np.arange(128)[:, None]
    i = np.arange(128)[None, :]
    causal = (j <= i)
    tri = np.where(causal, -1.0 / 16.0, 0.0).astype(np.float32)
    mask = causal.astype(np.float32)
    rdec = np.zeros((128, HR, 2, 128), dtype=np.float64)
    pos = np.arange(128, dtype=np.float64)
    for h in range(HR):
        lg = np.log1p(-(2.0 ** (-5.0 - h)))
        rdec[:, h, 0, :] = np.exp(lg * (pos + 1.0))[None, :]
        rdec[:, h, 1, :] = (np.exp(-lg * (pos + 1.0)) * (256.0 ** -0.5))[None, :]
    rdec = rdec.reshape(128, HR * 2 * 128).astype(np.float32)
    inv_freq = (np.float32(ROPE_BASE) ** (-np.arange(0, 256, 2, dtype=np.float32) / np.float32(256))).astype(np.float32)
    return ident, tri, mask, rdec, inv_freq


def make_in_maps(cfg, n_cores, x, positions, attn_norm, w_in, gla_gate_up, gla_gate_bias, gla_out_norm,
                 ret_norm_gain, ret_norm_bias, w_out, mlp_norm, w_up, w_down, final_norm):
    f = np.float32
    T = cfg.T
    ident, tri, mask, rdec, inv_freq = make_consts(cfg)
    bc = lambda a: np.ascontiguousarray(np.broadcast_to(np.asarray(a, f)[:, None, :], (a.shape[0], 128, a.shape[1])))
    gup = np.ascontiguousarray(np.concatenate([np.asarray(gla_gate_up, f), np.asarray(gla_gate_bias, f)[:, None, :]], axis=1))
    shared = {
        "w_in": np.ascontiguousarray(w_in, dtype=f), "gup": gup, "an": bc(attn_norm), "mn": bc(mlp_norm),
        "fn": np.ascontiguousarray(np.broadcast_to(np.asarray(final_norm, f)[None, :], (128, final_norm.shape[0]))),
        "gon": bc(gla_out_norm), "rng": bc(ret_norm_gain), "rnb": bc(ret_norm_bias),
        "w_out": np.ascontiguousarray(w_out, dtype=f), "w_up": np.ascontiguousarray(w_up, dtype=f),
        "w_down": np.ascontiguousarray(w_down, dtype=f),
        "ident": ident, "tri": tri, "mask": mask, "rdec": rdec,
    }
    x = np.asarray(x, f)
    positions = np.asarray(positions, np.int32)
    maps = []
    for cid in range(n_cores):
        b, half = cid // 2, cid % 2
        m = dict(shared)
        m["x"] = np.ascontiguousarray(x[b, half * T:(half + 1) * T, :])
        m["pos"] = np.ascontiguousarray(np.broadcast_to(positions[b, half * T:(half + 1) * T][None, :], (128, T)))
        misc = np.zeros((128, 4), f)
        misc[:, 0] = inv_freq
        misc[:, 1] = float(half)
        m["misc"] = misc
        maps.append(m)
    return maps


def kernel(x, positions, attn_norm, w_in, gla_gate_up, gla_gate_bias, gla_out_norm,
           ret_norm_gain, ret_norm_bias, w_out, mlp_norm, w_up, w_down, final_norm):
    cfg = Cfg()
    n = 8
    B, S, D = x.shape
    nc = build(cfg, use_cc=True, pair_groups=[[0, 1], [2, 3], [4, 5], [6, 7]])
    maps = make_in_maps(cfg, n, x, positions, attn_norm, w_in, gla_gate_up, gla_gate_bias, gla_out_norm,
                        ret_norm_gain, ret_norm_bias, w_out, mlp_norm, w_up, w_down, final_norm)
    res = run_bass_kernel_spmd(nc, maps, core_ids=list(range(n)))
    outp = np.empty((B, S, D), np.float32)
    for cid in range(n):
        b, half = cid // 2, cid % 2
        outp[b, half * cfg.T:(half + 1) * cfg.T, :] = res.results[cid]["out"]
    return outp
```

```python
import contextlib
import numpy as np
import concourse.bass as bass
import concourse.mybir as mybir
from concourse.bass_utils import run_bass_kernel_spmd

F32 = mybir.dt.float32
BF16 = mybir.dt.bfloat16
I32 = mybir.dt.int32
AF = mybir.ActivationFunctionType
ALU = mybir.AluOpType

EPS = 1e-6
ROPE_BASE = 10000.0
TWO_PI = 2.0 * np.pi
CW1 = float(np.float32(6.28125))
CW2 = float(np.float32(TWO_PI - 6.28125))
RND = 12582912.0


class Cfg:
    def __init__(self, D=2048, T=2048, HG=4, HR=4, DFF=8192, L=2, TBF=1024):
        self.D, self.T, self.HG, self.HR, self.DFF, self.L, self.TBF = D, T, HG, HR, DFF, L, TBF
        self.KC = D // 128
        self.NT = T // 128
        self.QKG, self.VG = HG * 128, HG * 256
        self.QKR, self.VR = HR * 256, HR * 256
        self.DMIX = self.VG + self.VR
        self.KM = self.DMIX // 128
        self.o_gq = 0
        self.o_gk = self.QKG
        self.o_gv = 2 * self.QKG
        self.o_gg = self.o_gv + self.VG
        self.o_gr = self.o_gg + self.VG
        self.o_rq = self.o_gr + 16
        self.o_rk = self.o_rq + self.QKR
        self.o_rv = self.o_rk + self.QKR
        self.o_rg = self.o_rv + self.VR
        self.DIN = self.o_rg + self.VR
        self.NB = 2 * HG + 4 * HR
        self.NKD = HG + 2 * HR
        self.NH = HG + HR


class Buf:
    __slots__ = ("w", "r", "excl")

    def __init__(self, excl=False):
        self.w = {}
        self.r = {}
        self.excl = excl


class Eng:
    def __init__(self, ctx, name, handle):
        self.h = handle
        self.key = "e_" + name
        self.sem = ctx.nc.alloc_semaphore("es_" + name)
        ctx.sems[self.key] = self.sem
        self.cnt = 0
        self.waited = {}


class DSem:
    def __init__(self, ctx, name):
        self.ctx = ctx
        self.name = name
        self.sub = {}

    def for_queue(self, q):
        d = self.sub.get(q.key)
        if d is None:
            d = _DSub(self.ctx, self.name + "_" + q.key)
            self.sub[q.key] = d
            self.ctx.dsubs.append(d)
        return d


class _DSub:
    def __init__(self, ctx, name):
        self.key = "d_" + name
        self.sem = ctx.nc.alloc_semaphore("ds_" + name)
        ctx.sems[self.key] = self.sem
        self.cnt = 0


def _merge(d, s):
    for k, v in s.items():
        if d.get(k, 0) < v:
            d[k] = v


class Ctx:
    def __init__(self, nc):
        self.nc = nc
        self.sems = {}
        self.pe = Eng(self, "pe", nc.tensor)
        self.act = Eng(self, "act", nc.scalar)
        self.dve = Eng(self, "dve", nc.vector)
        self.pool = Eng(self, "pool", nc.gpsimd)
        self.sp = Eng(self, "sp", nc.sync)
        self.engs = [self.pe, self.act, self.dve, self.pool, self.sp]
        self.dsems = []
        self.dsubs = []
        self.dnames = {}
        self.dcache = {}

    def dsem(self, name):
        i = self.dnames.get(name, 0)
        self.dnames[name] = i + 1
        key = "%s_%d" % (name, i)
        d = self.dcache.get(key)
        if d is None:
            d = self.dcache[key] = DSem(self, key)
            self.dsems.append(d)
        return d

    def new_layer(self):
        self.dnames = {}

    def _wait(self, eng, deps):
        for key, val in deps.items():
            if eng.waited.get(key, 0) >= val:
                continue
            eng.h.wait_ge(self.sems[key], val)
            eng.waited[key] = val

    def _deps(self, eng, reads, writes):
        if any(b.excl for b in reads):
            writes = list(writes) + [b for b in reads if b.excl]
            reads = [b for b in reads if not b.excl]
        d = {}
        for b in reads:
            _merge(d, b.w)
        for b in writes:
            w = dict(b.w)
            r = dict(b.r)
            _merge(d, w)
            _merge(d, r)
        if eng is self.pe:
            d.pop(eng.key, None)
        return d

    def _mark(self, key, cnt, reads, writes):
        if any(b.excl for b in reads):
            writes = list(writes) + [b for b in reads if b.excl]
            reads = [b for b in reads if not b.excl]
        for b in writes:
            b.w = {key: cnt}
            b.r = {}
        for b in reads:
            if not any(b is x for x in writes):
                if b.r.get(key, 0) < cnt:
                    b.r[key] = cnt

    def op(self, eng, fn, reads=(), writes=()):
        self._wait(eng, self._deps(eng, reads, writes))
        inst = fn(eng.h)
        eng.cnt += 1
        inst.then_inc(eng.sem, 1)
        self._mark(eng.key, eng.cnt, reads, writes)
        return inst

    def pe_group(self, items, reads=(), writes=()):
        eng = self.pe
        self._wait(eng, self._deps(eng, reads, writes))
        inst = None
        for it in items:
            if it[0] == "mm":
                inst = eng.h.matmul(it[1], it[2], it[3], start=it[4], stop=it[5])
            else:
                inst = eng.h.transpose(it[1], it[2], it[3])
        eng.cnt += 1
        inst.then_inc(eng.sem, 1)
        self._mark(eng.key, eng.cnt, reads, writes)

    def dma(self, q, out_ap, in_ap, ds, reads=(), writes=()):
        ds = ds.for_queue(q)
        deps = self._deps(q, reads, writes)
        deps.pop(ds.key, None)
        self._wait(q, deps)
        inst = q.h.dma_start(out=out_ap, in_=in_ap)
        ds.cnt += 16
        inst.then_inc(ds.sem, 16)
        self._mark(ds.key, ds.cnt, reads, writes)

    def barrier(self):
        allv = {e.key: e.cnt for e in self.engs if e.cnt}
        for d in self.dsubs:
            if d.cnt:
                allv[d.key] = d.cnt
        for e in self.engs:
            self._wait(e, {k: v for k, v in allv.items() if k != e.key})


class _Stop(Exception):
    pass


def build(cfg, use_cc=True, pair_groups=None, stop=None):
    c = cfg
    D, T, HG, HR, DFF, L = c.D, c.T, c.HG, c.HR, c.DFF, c.L
    KC, NT, KM, NB, NKD = c.KC, c.NT, c.KM, c.NB, c.NKD
    DMIX, VG, VR, QKG = c.DMIX, c.VG, c.VR, c.QKG
    nc = bass.Bass("TRN2", target_bir_lowering=False)
    K = Ctx(nc)
    pe, act, dve, pool, sp = K.pe, K.act, K.dve, K.pool, K.sp

    def din(name, shape, dt=F32):
        return nc.dram_tensor(name, list(shape), dt, kind="ExternalInput").ap()

    x_in = din("x", [T, D])
    pos_in = din("pos", [128, T], I32)
    w_in = din("w_in", [L, D, c.DIN])
    gup_in = din("gup", [L, 17, QKG])
    an_in = din("an", [L, 128, D])
    mn_in = din("mn", [L, 128, D])
    fn_in = din("fn", [128, D])
    gon_in = din("gon", [L, 128, VG])
    rng_in = din("rng", [L, 128, VR])
    rnb_in = din("rnb", [L, 128, VR])
    w_out = din("w_out", [L, DMIX, D])
    w_up = din("w_up", [L, D, DFF])
    w_down = din("w_down", [L, DFF, D])
    ident_in = din("ident", [128, 128])
    tri_in = din("tri", [128, 128])
    mask_in = din("mask", [128, 128])
    rdec_in = din("rdec", [128, HR * 2 * 128])
    misc_in = din("misc", [128, 4])
    out = nc.dram_tensor("out", [T, D], F32, kind="ExternalOutput").ap()

    xs = nc.dram_tensor("xs", [T, D], F32).ap()
    qk_s = nc.dram_tensor("qk_s", [128, NB, T], BF16).ap()
    kd_s = nc.dram_tensor("kd_s", [T, NKD * 128], BF16).ap()
    v_s = nc.dram_tensor("v_s", [T, DMIX], BF16).ap()
    sg_s = nc.dram_tensor("sg_s", [T, DMIX], F32).ap()
    cs_s = nc.dram_tensor("cs_s", [2, 128, T], F32).ap()
    cc_in_t = [nc.dram_tensor("cc_in%d" % l, [NKD * 128, 256], F32) for l in range(L)]
    cc_out_t = [nc.dram_tensor("cc_out%d" % l, [2 * NKD * 128, 256], F32) for l in range(L)]

    dbufs = {}

    def DB(*key):
        b = dbufs.get(key)
        if b is None:
            b = dbufs[key] = Buf()
        return b

    banks = [nc.alloc_psum_tensor("bank%d" % i, [128, 512], F32) for i in range(8)]
    bankb = [Buf(excl=True) for _ in range(8)]

    def bf(ap):
        return ap.bitcast(BF16)

    es0 = contextlib.ExitStack()

    sbn = [0]

    def sb(es, name, shape, dt):
        sbn[0] += 1
        return es.enter_context(nc.sbuf_tensor("%s_s%d" % (name, sbn[0]), list(shape), dt))

    ident_b = sb(es0, "ident_b", [128, 128], BF16)
    tri_f = sb(es0, "tri_f", [128, 128], F32)
    mask_f = sb(es0, "mask_f", [128, 128], F32)
    misc = sb(es0, "misc", [128, 4], F32)
    d_all = sb(es0, "d_all", [128, HG * NT], F32)
    stat = sb(es0, "stat", [128, 64], F32)
    cB = Buf()
    dB, S1B, SbB = Buf(), [Buf() for _ in range(NKD)], [Buf() for _ in range(NKD)]
    cs = K.dsem("const")
    K.dma(pool, ident_b[:], ident_in, cs, writes=[cB])
    K.dma(sp, tri_f[:], tri_in, cs, writes=[cB])
    K.dma(sp, mask_f[:], mask_in, cs, writes=[cB])
    K.dma(sp, misc[:], misc_in, cs, writes=[cB])

    gammas = {0.0: 1.0}
    hgam = [1.0 - 2.0 ** (-5.0 - h) for h in range(HR)]

    with contextlib.ExitStack() as es:
        cosT = sb(es, "cosT", [128, T], F32)
        sinT = sb(es, "sinT", [128, T], F32)
        posi = sb(es, "posi", [128, T], I32)
        ang = sb(es, "ang", [128, T], F32)
        t1 = sb(es, "rt1", [128, T], F32)
        t2 = sb(es, "rt2", [128, T], F32)
        pB, aB, t1B, t2B, cosB, sinB = Buf(), Buf(), Buf(), Buf(), Buf(), Buf()
        ds = K.dsem("pos")
        K.dma(sp, posi[:], pos_in, ds, writes=[pB])
        K.op(dve, lambda e: e.tensor_copy(t1[:], posi[:]), [pB], [t1B])
        K.op(dve, lambda e: e.tensor_scalar(ang[:], t1[:], misc[:, 0:1], None, op0=ALU.mult), [t1B, cB], [aB])
        for which, dst, dstB in (("sin", sinT, sinB), ("cos", cosT, cosB)):
            off = 0.0 if which == "sin" else 0.25
            K.op(dve, lambda e: e.tensor_scalar(t1[:], ang[:], 1.0 / TWO_PI, off, op0=ALU.mult, op1=ALU.add), [aB], [t1B])
            K.op(dve, lambda e: e.tensor_scalar(t2[:], t1[:], RND, None, op0=ALU.add), [t1B], [t2B])
            K.op(dve, lambda e: e.tensor_scalar(t2[:], t2[:], -RND, None, op0=ALU.add), [t2B], [t2B])
            K.op(dve, lambda e: e.scalar_tensor_tensor(out=t1[:], in0=t2[:], scalar=-CW1, in1=ang[:], op0=ALU.mult, op1=ALU.add), [t2B, aB], [t1B])
            K.op(dve, lambda e: e.scalar_tensor_tensor(out=t1[:], in0=t2[:], scalar=-CW2, in1=t1[:], op0=ALU.mult, op1=ALU.add), [t2B, t1B], [t1B])
            sh = 0.0 if which == "sin" else float(np.pi / 2)
            K.op(dve, lambda e: e.tensor_scalar(t2[:], t1[:], sh, 3.1415925, op0=ALU.add, op1=ALU.min), [t1B], [t2B])
            K.op(dve, lambda e: e.tensor_scalar(t1[:], t2[:], -3.1415925, None, op0=ALU.max), [t2B], [t1B])
            K.op(act, lambda e: e.activation(out=dst[:], in_=t1[:], func=AF.Sin), [t1B], [dstB])
        K.dma(sp, cs_s[0], cosT[:], K.dsem("cso"), reads=[cosB], writes=[DB("cs", 0)])
        K.dma(sp, cs_s[1], sinT[:], K.dsem("cso"), reads=[sinB], writes=[DB("cs", 1)])
        K.barrier()

    def rstd_from_ms(col_ms, col_out, rB):
        K.op(act, lambda e: e.activation(out=stat[:, col_out:col_out + 1], in_=stat[:, col_ms:col_ms + 1],
                                         func=AF.Ln, bias=EPS, scale=1.0), [rB], [rB])
        K.op(act, lambda e: e.activation(out=stat[:, col_out:col_out + 1], in_=stat[:, col_out:col_out + 1],
                                         func=AF.Exp, scale=-0.5), [rB], [rB])

    nstB = [Buf(), Buf()]

    def norm_to_T(xt, xB, gam, gamB, hT, hTB, tok0, h_bfs, hBs, junk, par, pbanks):
        c0 = 20 + 2 * par
        stB = nstB[par]
        h_bf, hB = h_bfs[par], hBs[par]
        K.op(act, lambda e: e.activation(out=junk[:], in_=xt, func=AF.Square, scale=float(D ** -0.5),
                                         accum_out=stat[:, c0:c0 + 1]), [xB], [stB])
        rstd_from_ms(c0, c0 + 1, stB)
        K.op(dve, lambda e: e.scalar_tensor_tensor(out=h_bf[:], in0=xt, scalar=stat[:, c0 + 1:c0 + 2], in1=gam,
                                                   op0=ALU.mult, op1=ALU.mult), [xB, stB, gamB], [hB])
        for gi, k0 in enumerate(range(0, KC, 4)):
            nk = min(4, KC - k0)
            pbk = pbanks[gi % len(pbanks)]
            pb = bf(banks[pbk][:])
            K.pe_group([("tr", pb[:, j * 128:(j + 1) * 128], h_bf[:, (k0 + j) * 128:(k0 + j + 1) * 128], ident_b[:])
                        for j in range(nk)], [hB, cB], [bankb[pbk]])
            src = pb[:, 0:nk * 128].rearrange("p (k t) -> p k t", k=nk)
            dst = hT[:, k0:k0 + nk, tok0:tok0 + 128]
            if gi % 2 == 0:
                K.op(act, lambda e: e.copy(dst, src), [bankb[pbk]], [hTB])
            else:
                K.op(dve, lambda e: e.tensor_copy(dst, src), [bankb[pbk]], [hTB])

    def chk(name):
        if stop == name:
            raise _Stop()

    def _layers():
        chk('rot')
        for l in range(L):
            K.new_layer()
            x_src = x_in if l == 0 else xs
            last = l == L - 1

            with contextlib.ExitStack() as es:
                hT = sb(es, "hT", [128, KC, T], BF16)
                hTB = [Buf() for _ in range(NT)]
                sp_all = sb(es, "sp_all", [128, NT, QKG], F32)
                spB = [Buf() for _ in range(NT)]
                stB = Buf()
                S1 = sb(es, "S1", [128, NKD, 256], F32)
                rdec = sb(es, "rdec", [128, HR * 2 * 128], F32)
                rotB = Buf()
                K.dma(sp, rdec[:], rdec_in, K.dsem("rdec"), writes=[rotB])
                K.op(dve, lambda e: e.memset(S1[:], 0.0), [], S1B)
                NWS = 3
                wring = [sb(es, "wA%d" % i, [128, KC, 512], BF16) for i in range(NWS)]
                wrB = [Buf() for _ in range(NWS)]
                wrds = [K.dsem("wA") for _ in range(NWS)]

                heads = [("g", h, c.o_gq + h * 128, c.o_gk + h * 128, c.o_gv + h * 256, c.o_gg + h * 256, 1) for h in range(HG)]
                heads += [("r", h, c.o_rq + h * 256, c.o_rk + h * 256, c.o_rv + h * 256, c.o_rg + h * 256, 2) for h in range(HR)]

                def load_set(i):
                    if i >= 2 * len(heads):
                        return
                    kind, h, cq, ck_, cv, cg, nblk = heads[i // 2]
                    s = i % NWS
                    if i % 2 == 0:
                        parts = ((cq, 128 * nblk, 0), (ck_, 128 * nblk, 256))
                    else:
                        parts = ((cv, 256, 0), (cg, 256, 256))
                    for (c0, n, dst) in parts:
                        K.dma(pool, wring[s][:, :, dst:dst + n],
                              w_in[l][:, c0:c0 + n].rearrange("(k p) c -> p k c", p=128), wrds[s], writes=[wrB[s]])

                load_set(0)
                load_set(1)

                with contextlib.ExitStack() as es2:
                    gam = sb(es2, "gamA", [128, D], F32)
                    gamB = Buf()
                    xt2 = [sb(es2, "xtA%d" % i, [128, D], F32) for i in range(2)]
                    xtB = [Buf(), Buf()]
                    xds = [K.dsem("xA"), K.dsem("xA")]
                    h_bf = [sb(es2, "h_bfA%d" % i, [128, D], BF16) for i in range(2)]
                    hB = [Buf(), Buf()]
                    junk = sb(es2, "junkA", [128, D], BF16)
                    K.dma(sp, gam[:], an_in[l], K.dsem("gamA"), writes=[gamB])
                    K.dma(sp, xt2[0][:], x_src[0:128, :], xds[0], writes=[xtB[0]])
                    for tt in range(NT):
                        s = tt % 2
                        if tt + 1 < NT:
                            K.dma(sp, xt2[1 - s][:], x_src[(tt + 1) * 128:(tt + 2) * 128, :], xds[1 - s], writes=[xtB[1 - s]])
                        norm_to_T(xt2[s][:], xtB[s], gam[:], gamB, hT, hTB[tt], tt * 128, h_bf, hB, junk, s, [0, 1])
                    K.barrier()
                chk('A0')

                with contextlib.ExitStack() as es2:
                    grT = sb(es2, "grT", [32, T], F32)
                    grB = Buf()
                    gup = sb(es2, "gup", [32, QKG], F32)
                    gupB = Buf()
                    wgr = sb(es2, "wgr", [128, KC, 16], BF16)
                    wgrB = Buf()
                    ez = sb(es2, "ez", [128, QKG], F32)
                    ezB = Buf()
                    K.op(dve, lambda e: e.memset(grT[:], 1.0), [], [grB])
                    K.dma(sp, gup[0:17, :], gup_in[l], K.dsem("gupA"), writes=[gupB])
                    K.dma(pool, wgr[:], w_in[l][:, c.o_gr:c.o_gr + 16].rearrange("(k p) c -> p k c", p=128), K.dsem("wgrA"), writes=[wgrB])
                    for tg in range(0, T, 512):
                        n = min(512, T - tg)
                        tiles = list(range(tg // 128, (tg + n) // 128))
                        K.pe_group([("mm", banks[1][0:16, 0:n], wgr[:, kc, :], hT[:, kc, tg:tg + n], kc == 0, kc == KC - 1)
                                    for kc in range(KC)], [wgrB] + [hTB[t] for t in tiles], [bankb[1]])
                        K.op(act, lambda e: e.copy(grT[0:16, tg:tg + n], banks[1][0:16, 0:n]), [bankb[1]], [grB])
                    for tt in range(NT):
                        K.pe_group([("mm", banks[2][:, 0:QKG], grT[0:17, tt * 128:(tt + 1) * 128], gup[0:17, :], True, True)],
                                   [grB, gupB], [bankb[2]])
                        K.op(act, lambda e: e.activation(out=ez[:], in_=banks[2][:, 0:QKG], func=AF.Exp, scale=-1.0), [bankb[2]], [ezB])
                        K.op(act, lambda e: e.activation(out=sp_all[:, tt, :], in_=ez[:], func=AF.Ln, bias=1.0, scale=1.0), [ezB], [spB[tt]])
                    K.barrier()
                chk('A1')

                cosT = sb(es, "cosT", [128, T], F32)
                sinT = sb(es, "sinT", [128, T], F32)
                K.dma(sp, cosT[:], cs_s[0], K.dsem("cos"), reads=[DB("cs", 0)], writes=[rotB])
                K.dma(sp, sinT[:], cs_s[1], K.dsem("sin"), reads=[DB("cs", 1)], writes=[rotB])
                eb = sb(es, "eb", [128, 512], F32)
                enb = sb(es, "enb", [128, 512], F32)
                ebB, enbB = Buf(), Buf()
                qf = [sb(es, "qf%d" % i, [128, 512], F32) for i in range(2)]
                qfB = [Buf(), Buf()]
                rt = [sb(es, "rtA%d" % i, [128, 512], F32) for i in range(4)]
                rtB = [Buf() for _ in range(4)]
                fm = [sb(es, "fm%d" % i, [128, 512], BF16) for i in range(4)]
                fmB = [Buf() for _ in range(4)]
                fmds = [K.dsem("fm") for _ in range(4)]
                kdt = [sb(es, "kdt%d" % i, [128, 256], BF16) for i in range(4)]
                kdtB = [Buf() for _ in range(4)]
                kdds = [K.dsem("kd") for _ in range(4)]
                vt = [sb(es, "vt%d" % i, [128, 256], BF16) for i in range(4)]
                vtB = [Buf() for _ in range(4)]
                vds = [K.dsem("v") for _ in range(4)]
                sgt = [sb(es, "sgt%d" % i, [128, 256], F32) for i in range(4)]
                sgtB = [Buf() for _ in range(4)]
                sgds = [K.dsem("sg") for _ in range(4)]

                fmi = 0
                tok_i = [0]
                for hi, (kind, h, cq, ck, cv, cg, nblk) in enumerate(heads):
                    if hi == 1:
                        chk('A2f')
                    load_set(2 * hi + 2)
                    wt, wtB = wring[(2 * hi) % NWS], wrB[(2 * hi) % NWS]
                    wv, wvB = wring[(2 * hi + 1) % NWS], wrB[(2 * hi + 1) % NWS]
                    sblk0 = h if kind == "g" else HG + 2 * h
                    qblk0 = 2 * h if kind == "g" else 2 * HG + 4 * h
                    for tg in range(0, T, 512):
                        n = min(512, T - tg)
                        tiles = list(range(tg // 128, (tg + n) // 128))
                        hr = [hTB[t] for t in tiles]
                        if kind == "g":
                            K.pe_group([("mm", banks[1][:, j * 128:(j + 1) * 128], sp_all[:, t, h * 128:(h + 1) * 128], tri_f[:], True, True)
                                        for j, t in enumerate(tiles)], [spB[t] for t in tiles] + [cB], [bankb[1]])
                            K.op(act, lambda e: e.activation(out=eb[:, 0:n], in_=banks[1][:, 0:n], func=AF.Exp), [bankb[1]], [ebB])
                            K.op(act, lambda e: e.activation(out=enb[:, 0:n], in_=banks[1][:, 0:n], func=AF.Exp, scale=-1.0), [bankb[1]], [enbB])
                            for j, t in enumerate(tiles):
                                K.op(dve, lambda e: e.tensor_copy(d_all[:, h * NT + t:h * NT + t + 1], eb[:, j * 128 + 127:j * 128 + 128]), [ebB], [dB])
                        chk('A2a')
                        for qk in range(2):
                            wcol = 0 if qk == 0 else 256
                            for blk in range(nblk):
                                bk = 2 + (qk * 2 + blk) % 2 if kind == "g" else 2 + blk
                                pbk = banks[bk]
                                K.pe_group([("mm", pbk[:, 0:n], wt[:, kc, wcol + blk * 128:wcol + (blk + 1) * 128], hT[:, kc, tg:tg + n], kc == 0, kc == KC - 1)
                                            for kc in range(KC)], [wtB] + hr, [bankb[bk]])
                                if kind == "g":
                                    f = fmi % 4
                                    fmi += 1
                                    if qk == 0:
                                        K.op(dve, lambda e: e.scalar_tensor_tensor(out=fm[f][:, 0:n], in0=pbk[:, 0:n], scalar=float(128 ** -0.5), in1=eb[:, 0:n],
                                                                                   op0=ALU.mult, op1=ALU.mult), [bankb[bk], ebB], [fmB[f]])
                                    else:
                                        K.op(dve, lambda e: e.tensor_tensor(out=fm[f][:, 0:n], in0=pbk[:, 0:n], in1=enb[:, 0:n], op=ALU.mult), [bankb[bk], enbB], [fmB[f]])
                                    K.dma(sp, qk_s[:, qblk0 + qk, tg:tg + n], fm[f][:, 0:n], fmds[f], reads=[fmB[f]], writes=[DB("qk", qblk0 + qk, tg)])
                                    if qk == 1:
                                        kf = [f]
                                else:
                                    dec = rdec[:, (h * 2 + qk) * 128:(h * 2 + qk + 1) * 128]
                                    for j in range(n // 128):
                                        K.op(dve, lambda e: e.tensor_tensor(out=qf[blk][:, j * 128:(j + 1) * 128], in0=pbk[:, j * 128:(j + 1) * 128], in1=dec, op=ALU.mult),
                                             [bankb[bk], rotB], [qfB[blk]])
                            if kind == "r":
                                fs = []
                                for blk in range(2):
                                    a, b_ = (qf[0], qf[1]) if blk == 0 else (qf[1], qf[0])
                                    aB_, bB_ = (qfB[0], qfB[1]) if blk == 0 else (qfB[1], qfB[0])
                                    r0, r1 = rt[2 * blk], rt[2 * blk + 1]
                                    r0B, r1B = rtB[2 * blk], rtB[2 * blk + 1]
                                    K.op(pool, lambda e: e.tensor_tensor(out=r0[:, 0:n], in0=a[:, 0:n], in1=cosT[:, tg:tg + n], op=ALU.mult), [aB_, rotB], [r0B])
                                    K.op(pool, lambda e: e.tensor_tensor(out=r1[:, 0:n], in0=b_[:, 0:n], in1=sinT[:, tg:tg + n], op=ALU.mult), [bB_, rotB], [r1B])
                                    f = fmi % 4
                                    fmi += 1
                                    K.op(dve, lambda e: e.tensor_tensor(out=fm[f][:, 0:n], in0=r0[:, 0:n], in1=r1[:, 0:n],
                                                                        op=(ALU.subtract if blk == 0 else ALU.add)), [r0B, r1B], [fmB[f]])
                                    bi = qblk0 + qk * 2 + blk
                                    K.dma(sp, qk_s[:, bi, tg:tg + n], fm[f][:, 0:n], fmds[f], reads=[fmB[f]], writes=[DB("qk", bi, tg)])
                                    fs.append(f)
                                if qk == 1:
                                    kf = fs
                        if tg + 512 >= T:
                            load_set(2 * hi + 3)
                        chk('A2b')
                        vcol = (h * 256) if kind == "g" else (VG + h * 256)
                        slot = {}

                        def e_vg(j, t):
                            si = tok_i[0] % 4
                            bk = (5, 6, 0)[tok_i[0] % 3]
                            tok_i[0] += 1
                            slot[j] = si
                            K.pe_group([("mm", banks[bk][:, :], hT[:, kc, t * 128:(t + 1) * 128], wv[:, kc, 0:512], kc == 0, kc == KC - 1) for kc in range(KC)],
                                       [wvB, hTB[t]], [bankb[bk]])
                            K.op(dve, lambda e: e.tensor_copy(vt[si][:], banks[bk][:, 0:256]), [bankb[bk]], [vtB[si]])
                            K.op(act, lambda e: e.activation(out=sgt[si][:], in_=banks[bk][:, 256:512], func=AF.Silu), [bankb[bk]], [sgtB[si]])
                            K.dma(sp, v_s[t * 128:(t + 1) * 128, vcol:vcol + 256], vt[si][:], vds[si], reads=[vtB[si]], writes=[DB("v", vcol, t)])
                            K.dma(sp, sg_s[t * 128:(t + 1) * 128, vcol:vcol + 256], sgt[si][:], sgds[si], reads=[sgtB[si]], writes=[DB("sg", vcol, t)])

                        def e_trl(j, t):
                            si = slot[j]
                            pb = bf(banks[4][:])
                            K.pe_group([("tr", pb[:, b2 * 128:(b2 + 1) * 128], fm[kf[b2]][:, j * 128:(j + 1) * 128], ident_b[:]) for b2 in range(nblk)],
                                       [fmB[x] for x in kf] + [cB], [bankb[4]])
                            K.op(act, lambda e: e.copy(kdt[si][:, 0:nblk * 128], pb[:, 0:nblk * 128]), [bankb[4]], [kdtB[si]])
                            K.dma(sp, kd_s[t * 128:(t + 1) * 128, sblk0 * 128:(sblk0 + nblk) * 128], kdt[si][:, 0:nblk * 128], kdds[si],
                                  reads=[kdtB[si]], writes=[DB("kd", sblk0, t)])

                        def e_local(j, t):
                            si = slot[j]
                            lb = 1 if (j % 2 == (1 if kind == "r" else 0)) else 7
                            for b2 in range(nblk):
                                K.pe_group([("mm", banks[lb][:, b2 * 256:(b2 + 1) * 256], kdt[si][:, b2 * 128:(b2 + 1) * 128], vt[si][:], True, True)],
                                           [kdtB[si], vtB[si]], [bankb[lb]])
                            for b2 in range(nblk):
                                sbk = sblk0 + b2
                                dsc = d_all[:, h * NT + t:h * NT + t + 1] if kind == "g" else float(hgam[h] ** 128)
                                K.op(dve, lambda e: e.tensor_scalar(S1[:, sbk, :], S1[:, sbk, :], dsc, None, op0=ALU.mult), [S1B[sbk], dB], [S1B[sbk]])
                                K.op(dve, lambda e: e.scalar_tensor_tensor(out=S1[:, sbk, :], in0=banks[lb][:, b2 * 256:(b2 + 1) * 256], scalar=dsc, in1=S1[:, sbk, :],
                                                                           op0=ALU.mult, op1=ALU.add), [bankb[lb], S1B[sbk], dB], [S1B[sbk]])

                        AHEAD = 3
                        ntl = len(tiles)
                        for j in range(min(AHEAD, ntl)):
                            e_vg(j, tiles[j])
                        for j in range(ntl):
                            e_trl(j, tiles[j])
                            if j + AHEAD < ntl:
                                e_vg(j + AHEAD, tiles[j + AHEAD])
                            e_local(j, tiles[j])
                chk('A2')
                ccds = K.dsem("cc")
                ccB, ccoB = Buf(), Buf()
                K.dma(sp, cc_in_t[l].ap().rearrange("(b p) v -> p b v", p=128), S1[:], ccds, reads=S1B, writes=[ccB])
                K.barrier()

            chk('A')
            with contextlib.ExitStack() as es:
                S1 = sb(es, "S1b", [128, NKD, 256], F32)
                Sb = sb(es, "Sb", [128, NKD, 256], BF16)
                wo = sb(es, "wo", [128, KM, D], BF16)
                woB = Buf()
                gon = sb(es, "gon", [128, VG], F32)
                rgn = sb(es, "rgn", [128, VR], F32)
                rbn = sb(es, "rbn", [128, VR], F32)
                nB = Buf()
                qk2 = [sb(es, "qkB%d" % i, [128, NB, 128], BF16) for i in range(2)]
                kd2 = [sb(es, "kdB%d" % i, [128, NKD * 128], BF16) for i in range(2)]
                v2 = [sb(es, "vB%d" % i, [128, DMIX], BF16) for i in range(2)]
                sg2 = [sb(es, "sgB%d" % i, [128, DMIX], F32) for i in range(2)]
                x2 = [sb(es, "xB%d" % i, [128, D], F32) for i in range(2)]
                inB = [Buf(), Buf()]
                xB = [Buf(), Buf()]
                inds = [K.dsem("inB"), K.dsem("inB")]
                xds = [K.dsem("xB"), K.dsem("xB")]
                xods = [K.dsem("xoB"), K.dsem("xoB")]
                sc = [sb(es, "sc%d" % i, [128, 128], BF16) for i in range(2)]
                scB = [Buf(), Buf()]
                tn = [sb(es, "tn%d" % i, [128, 256], F32) for i in range(2)]
                tnB = [Buf(), Buf()]
                junk = sb(es, "junkB", [128, 256], BF16)
                mix = [sb(es, "mix%d" % i, [128, DMIX], BF16) for i in range(2)]
                mixB = [Buf(), Buf()]
                mixT = sb(es, "mixT", [128, KM, 128], BF16)
                mixTB = Buf()
                stB = Buf()

                cds = K.dsem("cB")
                for g4 in range(0, D, 512):
                    n4 = min(512, D - g4)
                    K.dma(pool, wo[:, :, g4:g4 + n4], w_out[l][:, g4:g4 + n4].rearrange("(k p) c -> p k c", p=128), cds, writes=[woB])
                K.dma(sp, gon[:], gon_in[l], cds, writes=[nB])
                K.dma(sp, rgn[:], rng_in[l], cds, writes=[nB])
                K.dma(sp, rbn[:], rnb_in[l], cds, writes=[nB])
                if use_cc:
                    ccsem = nc.alloc_semaphore("ccs%d" % l)
                    nc.gpsimd.collective_compute("AllGather", ALU.bypass, replica_groups=pair_groups,
                                                 ins=[cc_in_t[l].ap().opt()], outs=[cc_out_t[l].ap().opt()]).then_inc(ccsem)
                    nc.gpsimd.wait_ge(ccsem, 1)
                    src = cc_out_t[l].ap()[0:NKD * 128, :]
                else:
                    src = cc_in_t[l].ap()
                K.dma(pool, S1[:], src.rearrange("(b p) v -> p b v", p=128), K.dsem("ccl"), reads=[ccB], writes=S1B)
                K.op(dve, lambda e: e.tensor_scalar(S1[:], S1[:], misc[:, 1:2], None, op0=ALU.mult), S1B + [cB], S1B)
                K.op(dve, lambda e: e.tensor_copy(Sb[:], S1[:]), S1B, SbB)

                def load_tile(t):
                    s = t % 2
                    tk = slice(t * 128, (t + 1) * 128)
                    K.dma(sp, qk2[s][:], qk_s[:, :, tk], inds[s], reads=[DB("qk", b_, (t // 4) * 512) for b_ in range(NB)], writes=[inB[s]])
                    K.dma(sp, kd2[s][:], kd_s[tk, :], inds[s], reads=[DB("kd", b_, t) for b_ in range(NKD)], writes=[inB[s]])
                    K.dma(sp, v2[s][:], v_s[tk, :], inds[s], reads=[DB("v", cc_, t) for cc_ in range(0, DMIX, 256)], writes=[inB[s]])
                    K.dma(sp, sg2[s][:], sg_s[tk, :], inds[s], reads=[DB("sg", cc_, t) for cc_ in range(0, DMIX, 256)], writes=[inB[s]])

                def load_x(t):
                    s = t % 2
                    tk = slice(t * 128, (t + 1) * 128)
                    K.dma(sp, x2[s][:], x_src[tk, :], xds[s], reads=[DB("xs", t)] if l > 0 else [], writes=[xB[s]])

                hi_all = [("g", h) for h in range(HG)] + [("r", h) for h in range(HR)]
                NHh = len(hi_all)

                def hinfo(idx):
                    kind, h = hi_all[idx]
                    nblk = 1 if kind == "g" else 2
                    sblk0 = h if kind == "g" else HG + 2 * h
                    qb0 = 2 * h if kind == "g" else 2 * HG + 4 * h
                    vcol = (h * 256) if kind == "g" else (VG + h * 256)
                    return kind, h, nblk, sblk0, qb0, vcol, idx % 2

                def e_scores(t, idx):
                    kind, h, nblk, sblk0, qb0, vcol, i2 = hinfo(idx)
                    s = t % 2
                    K.pe_group([("mm", banks[i2][:, 0:128], qk2[s][:, qb0 + nblk + b2, :], qk2[s][:, qb0 + b2, :], b2 == 0, b2 == nblk - 1) for b2 in range(nblk)],
                               [inB[s]], [bankb[i2]])
                    K.op(dve, lambda e: e.tensor_tensor(out=sc[i2][:], in0=banks[i2][:, 0:128], in1=mask_f[:], op=ALU.mult), [bankb[i2], cB], [scB[i2]])

                def e_head(t, idx):
                    kind, h, nblk, sblk0, qb0, vcol, i2 = hinfo(idx)
                    s = t % 2
                    pbo, pbl = 2 + i2, 4 + i2
                    items = [("mm", banks[pbo][:, 0:256], sc[i2][:], v2[s][:, vcol:vcol + 256], True, False)]
                    for b2 in range(nblk):
                        items.append(("mm", banks[pbo][:, 0:256], qk2[s][:, qb0 + b2, :], Sb[:, sblk0 + b2, :], False, b2 == nblk - 1))
                    K.pe_group(items, [scB[i2], inB[s]] + [SbB[sblk0 + b2] for b2 in range(nblk)], [bankb[pbo]])
                    for b2 in range(nblk):
                        K.pe_group([("mm", banks[pbl][:, b2 * 256:(b2 + 1) * 256], kd2[s][:, (sblk0 + b2) * 128:(sblk0 + b2 + 1) * 128], v2[s][:, vcol:vcol + 256], True, True)],
                                   [inB[s]], [bankb[pbl]])
                    for b2 in range(nblk):
                        sbk = sblk0 + b2
                        if kind == "g":
                            dsc = d_all[:, h * NT + t:h * NT + t + 1]
                        else:
                            dsc = float(hgam[h] ** 128)
                        K.op(act, lambda e: e.activation(out=S1[:, sbk, :], in_=S1[:, sbk, :], func=AF.Identity, scale=dsc), [S1B[sbk], dB], [S1B[sbk]])
                        K.op(dve, lambda e: e.scalar_tensor_tensor(out=S1[:, sbk, :], in0=banks[pbl][:, b2 * 256:(b2 + 1) * 256], scalar=dsc, in1=S1[:, sbk, :],
                                                                   op0=ALU.mult, op1=ALU.add), [bankb[pbl], S1B[sbk], dB], [S1B[sbk]])
                        K.op(pool, lambda e: e.tensor_copy(Sb[:, sbk, :], S1[:, sbk, :]), [S1B[sbk]], [SbB[sbk]])
                    po = banks[pbo][:, 0:256]
                    c0 = 2 + 6 * i2
                    if kind == "g":
                        K.op(act, lambda e: e.activation(out=junk[:], in_=po, func=AF.Square, scale=float(256 ** -0.5), accum_out=stat[:, c0:c0 + 1]), [bankb[pbo]], [stB])
                        rstd_from_ms(c0, c0 + 1, stB)
                        K.op(dve, lambda e: e.scalar_tensor_tensor(out=tn[i2][:], in0=po, scalar=stat[:, c0 + 1:c0 + 2], in1=gon[:, h * 256:(h + 1) * 256],
                                                                   op0=ALU.mult, op1=ALU.mult), [bankb[pbo], stB, nB], [tnB[i2]])
                    else:
                        K.op(act, lambda e: e.activation(out=junk[:], in_=po, func=AF.Identity, scale=float(1.0 / 256), accum_out=stat[:, c0:c0 + 1]), [bankb[pbo]], [stB])
                        K.op(act, lambda e: e.activation(out=junk[:], in_=po, func=AF.Square, scale=float(256 ** -0.5), accum_out=stat[:, c0 + 1:c0 + 2]), [bankb[pbo]], [stB])
                        K.op(dve, lambda e: e.tensor_tensor(out=stat[:, c0 + 2:c0 + 3], in0=stat[:, c0:c0 + 1], in1=stat[:, c0:c0 + 1], op=ALU.mult), [stB], [stB])
                        K.op(dve, lambda e: e.tensor_tensor(out=stat[:, c0 + 3:c0 + 4], in0=stat[:, c0 + 1:c0 + 2], in1=stat[:, c0 + 2:c0 + 3], op=ALU.subtract), [stB], [stB])
                        rstd_from_ms(c0 + 3, c0 + 4, stB)
                        K.op(dve, lambda e: e.scalar_tensor_tensor(out=tn[i2][:], in0=po, scalar=stat[:, c0:c0 + 1], in1=rgn[:, h * 256:(h + 1) * 256],
                                                                   op0=ALU.subtract, op1=ALU.mult), [bankb[pbo], stB, nB], [tnB[i2]])
                        K.op(dve, lambda e: e.scalar_tensor_tensor(out=tn[i2][:], in0=tn[i2][:], scalar=stat[:, c0 + 4:c0 + 5], in1=rbn[:, h * 256:(h + 1) * 256],
                                                                   op0=ALU.mult, op1=ALU.add), [tnB[i2], stB, nB], [tnB[i2]])
                    K.op(pool, lambda e: e.tensor_tensor(out=mix[s][:, vcol:vcol + 256], in0=tn[i2][:], in1=sg2[s][:, vcol:vcol + 256], op=ALU.mult), [tnB[i2], inB[s]], [mixB[s]])

                def o_pieces(t):
                    s = t % 2
                    pl = []
                    for k0 in range(0, KM, 4):
                        def tr_piece(k0=k0):
                            nk = min(4, KM - k0)
                            pb = bf(banks[6][:])
                            K.pe_group([("tr", pb[:, j * 128:(j + 1) * 128], mix[s][:, (k0 + j) * 128:(k0 + j + 1) * 128], ident_b[:]) for j in range(nk)], [mixB[s], cB], [bankb[6]])
                            K.op(act, lambda e: e.copy(mixT[:, k0:k0 + nk, :], pb[:, 0:nk * 128].rearrange("p (k t) -> p k t", k=nk)), [bankb[6]], [mixTB])
                        pl.append(tr_piece)
                    ng = len(range(0, D, 512))
                    for gi, g4 in enumerate(range(0, D, 512)):
                        def op_piece(gi=gi, g4=g4):
                            n = min(512, D - g4)
                            K.pe_group([("mm", banks[7][:, 0:n], mixT[:, km, :], wo[:, km, g4:g4 + n], km == 0, km == KM - 1) for km in range(KM)], [mixTB, woB], [bankb[7]])
                            K.op(dve, lambda e: e.tensor_tensor(out=x2[s][:, g4:g4 + n], in0=banks[7][:, 0:n], in1=x2[s][:, g4:g4 + n], op=ALU.add), [bankb[7], xB[s]], [xB[s]])
                            if gi == ng - 1:
                                K.dma(sp, xs[t * 128:(t + 1) * 128, :], x2[s][:], xods[s], reads=[xB[s]], writes=[DB("xs", t)])
                        pl.append(op_piece)
                    return pl

                load_tile(0)
                for t in range(NT + 1):
                    if t + 1 < NT:
                        load_tile(t + 1)
                    if t < NT:
                        load_x(t)
                    pl = o_pieces(t - 1) if t >= 1 else []
                    npc = len(pl)
                    if t < NT:
                        e_scores(t, 0)
                        for idx in range(NHh):
                            if idx + 1 < NHh:
                                e_scores(t, idx + 1)
                            e_head(t, idx)
                            for pc in pl[idx * npc // NHh:(idx + 1) * npc // NHh]:
                                pc()
                    else:
                        for pc in pl:
                            pc()
                K.barrier()

            chk('B1')
            with contextlib.ExitStack() as es:
                TBF = min(c.TBF, T)
                NTB = TBF // 128
                FG = 512
                gam = sb(es, "gamF", [128, D], F32)
                gamB = Buf()
                xg = sb(es, "xg", [128, NTB, D], F32)
                xgB = [Buf() for _ in range(NTB)]
                xgds = [K.dsem("xg") for _ in range(NTB)]
                xods = [K.dsem("xgo") for _ in range(NTB)]
                h2T = sb(es, "h2T", [128, KC, TBF], BF16)
                h2TB = [Buf() for _ in range(NTB)]
                h_bf = [sb(es, "h_bfF%d" % i, [128, D], BF16) for i in range(2)]
                hB = [Buf(), Buf()]
                junk = sb(es, "junkF", [128, D], BF16)
                stB = Buf()
                wu = [sb(es, "wu%d" % i, [128, KC, FG], BF16) for i in range(2)]
                wuB = [Buf(), Buf()]
                wuds = [K.dsem("wu"), K.dsem("wu")]
                wd = [sb(es, "wd%d" % i, [128, FG // 128, D], BF16) for i in range(2)]
                wdB = [Buf(), Buf()]
                wdds = [K.dsem("wd"), K.dsem("wd")]
                uT = [sb(es, "uT%d" % i, [128, FG // 128, TBF], BF16) for i in range(2)]
                uTB = [Buf(), Buf()]
                sq = [sb(es, "sq%d" % i, [128, 512], F32) for i in range(2)]
                sqB = [Buf(), Buf()]
                gds = K.dsem("gF")
                NFG = DFF // FG
                nload = [0]

                def load_w(fi, bi=0):
                    s = (bi * NFG + fi) % 2
                    K.dma(pool, wu[s][:], w_up[l][:, fi * FG:(fi + 1) * FG].rearrange("(k p) c -> p k c", p=128), wuds[s], writes=[wuB[s]])
                    K.dma(pool, wd[s][:], w_down[l][fi * FG:(fi + 1) * FG, :].rearrange("(k p) c -> p k c", p=128), wdds[s], writes=[wdB[s]])

                def load_xg(tb_, j):
                    t_ = tb_ // 128 + j
                    K.dma(sp, xg[:, j, :], xs[t_ * 128:(t_ + 1) * 128, :], xgds[j], reads=[DB("xs", t_)], writes=[xgB[j]])

                nblk_f = T // TBF
                for bi, tb in enumerate(range(0, T, TBF)):
                    has_next = bi + 1 < nblk_f
                    if bi == 0:
                        load_w(0)
                        for j in range(NTB):
                            load_xg(tb, j)
                    if bi == 0 or last:
                        K.dma(sp, gam[:], mn_in[l], gds, writes=[gamB])
                    ev = 0
                    for fi in range(NFG):
                        s = (bi * NFG + fi) % 2
                        if fi + 1 < NFG:
                            load_w(fi + 1, bi)
                        elif has_next:
                            load_w(0, bi + 1)
                        for tg in range(0, TBF, 512):
                            n = min(512, TBF - tg)
                            if fi == 0:
                                for j in range(tg // 128, (tg + n) // 128):
                                    norm_to_T(xg[:, j, :], xgB[j], gam[:], gamB, h2T, h2TB[j], j * 128, h_bf, hB, junk, j % 2, [0, 7])
                            for fc in range(FG // 128):
                                e2 = ev % 2
                                ev += 1
                                bk = 1 + e2
                                K.pe_group([("mm", banks[bk][:, 0:n], wu[s][:, kc, fc * 128:(fc + 1) * 128], h2T[:, kc, tg:tg + n], kc == 0, kc == KC - 1) for kc in range(KC)],
                                           [wuB[s]] + h2TB[tg // 128:(tg + n) // 128], [bankb[bk]])
                                K.op(act, lambda e: e.activation(out=sq[e2][:, 0:n], in_=banks[bk][:, 0:n], func=AF.Square), [bankb[bk]], [sqB[e2]])
                                K.op(dve, lambda e: e.scalar_tensor_tensor(out=uT[s][:, fc, tg:tg + n], in0=banks[bk][:, 0:n], scalar=0.0, in1=sq[e2][:, 0:n],
                                                                           op0=ALU.is_gt, op1=ALU.mult), [bankb[bk], sqB[e2]], [uTB[s]])
                        di = 0
                        for j in range(NTB):
                            for g4 in range(0, D, 512):
                                n = min(512, D - g4)
                                bk = 3 + di % 4
                                di += 1
                                K.pe_group([("mm", banks[bk][:, 0:n], uT[s][:, fc, j * 128:(j + 1) * 128], wd[s][:, fc, g4:g4 + n], fc == 0, fc == FG // 128 - 1) for fc in range(FG // 128)],
                                           [uTB[s], wdB[s]], [bankb[bk]])
                                K.op(dve, lambda e: e.tensor_tensor(out=xg[:, j, g4:g4 + n], in0=banks[bk][:, 0:n], in1=xg[:, j, g4:g4 + n], op=ALU.add), [bankb[bk], xgB[j]], [xgB[j]])
                            if fi == NFG - 1 and not last:
                                t = tb // 128 + j
                                K.dma(sp, xs[t * 128:(t + 1) * 128, :], xg[:, j, :], xods[j], reads=[xgB[j]], writes=[DB("xs", t)])
                                if has_next:
                                    load_xg(tb + TBF, j)
                    if last:
                        K.dma(sp, gam[:], fn_in, gds, writes=[gamB])
                    for j in range(NTB):
                        t = tb // 128 + j
                        if not last:
                            pass
                        else:
                            K.op(act, lambda e: e.activation(out=junk[:], in_=xg[:, j, :], func=AF.Square, scale=float(D ** -0.5), accum_out=stat[:, 0:1]), [xgB[j]], [stB])
                            rstd_from_ms(0, 1, stB)
                            K.op(dve, lambda e: e.scalar_tensor_tensor(out=xg[:, j, :], in0=xg[:, j, :], scalar=stat[:, 1:2], in1=gam[:], op0=ALU.mult, op1=ALU.mult),
                                 [xgB[j], stB, gamB], [xgB[j]])
                            K.dma(sp, out[t * 128:(t + 1) * 128, :], xg[:, j, :], xods[j], reads=[xgB[j]], writes=[DB("out", t)])
                            if has_next:
                                load_xg(tb + TBF, j)
                K.barrier()

    try:
        _layers()
        K.barrier()
        es0.close()
    except _Stop:
        K.barrier()
    return nc


def make_consts(cfg):
    HR = cfg.HR
    ident = np.eye(128, dtype=np.float32)
    j = np.arange(128)[:, None]
    i = np.arange(128)[None, :]
    causal = (j <= i)
    tri = np.where(causal, -1.0 / 16.0, 0.0).astype(np.float32)
    mask = causal.astype(np.float32)
    rdec = np.zeros((128, HR, 2, 128), dtype=np.float64)
    pos = np.arange(128, dtype=np.float64)
    for h in range(HR):
        lg = np.log1p(-(2.0 ** (-5.0 - h)))
        rdec[:, h, 0, :] = np.exp(lg * (pos + 1.0))[None, :]
        rdec[:, h, 1, :] = (np.exp(-lg * (pos + 1.0)) * (256.0 ** -0.5))[None, :]
    rdec = rdec.reshape(128, HR * 2 * 128).astype(np.float32)
    inv_freq = (np.float32(ROPE_BASE) ** (-np.arange(0, 256, 2, dtype=np.float32) / np.float32(256))).astype(np.float32)
    return ident, tri, mask, rdec, inv_freq


def make_in_maps(cfg, n_cores, x, positions, attn_norm, w_in, gla_gate_up, gla_gate_bias, gla_out_norm,
                 ret_norm_gain, ret_norm_bias, w_out, mlp_norm, w_up, w_down, final_norm):
    f = np.float32
    T = cfg.T
    ident, tri, mask, rdec, inv_freq = make_consts(cfg)
    bc = lambda a: np.ascontiguousarray(np.broadcast_to(np.asarray(a, f)[:, None, :], (a.shape[0], 128, a.shape[1])))
    gup = np.ascontiguousarray(np.concatenate([np.asarray(gla_gate_up, f), np.asarray(gla_gate_bias, f)[:, None, :]], axis=1))
    shared = {
        "w_in": np.ascontiguousarray(w_in, dtype=f), "gup": gup, "an": bc(attn_norm), "mn": bc(mlp_norm),
        "fn": np.ascontiguousarray(np.broadcast_to(np.asarray(final_norm, f)[None, :], (128, final_norm.shape[0]))),
        "gon": bc(gla_out_norm), "rng": bc(ret_norm_gain), "rnb": bc(ret_norm_bias),
        "w_out": np.ascontiguousarray(w_out, dtype=f), "w_up": np.ascontiguousarray(w_up, dtype=f),
        "w_down": np.ascontiguousarray(w_down, dtype=f),
        "ident": ident, "tri": tri, "mask": mask, "rdec": rdec,
    }
    x = np.asarray(x, f)
    positions = np.asarray(positions, np.int32)
    maps = []
    for cid in range(n_cores):
        b, half = cid // 2, cid % 2
        m = dict(shared)
        m["x"] = np.ascontiguousarray(x[b, half * T:(half + 1) * T, :])
        m["pos"] = np.ascontiguousarray(np.broadcast_to(positions[b, half * T:(half + 1) * T][None, :], (128, T)))
        misc = np.zeros((128, 4), f)
        misc[:, 0] = inv_freq
        misc[:, 1] = float(half)
        m["misc"] = misc
        maps.append(m)
    return maps


def kernel(x, positions, attn_norm, w_in, gla_gate_up, gla_gate_bias, gla_out_norm,
           ret_norm_gain, ret_norm_bias, w_out, mlp_norm, w_up, w_down, final_norm):
    cfg = Cfg()
    n = 8
    B, S, D = x.shape
    nc = build(cfg, use_cc=True, pair_groups=[[0, 1], [2, 3], [4, 5], [6, 7]])
    maps = make_in_maps(cfg, n, x, positions, attn_norm, w_in, gla_gate_up, gla_gate_bias, gla_out_norm,
                        ret_norm_gain, ret_norm_bias, w_out, mlp_norm, w_up, w_down, final_norm)
    res = run_bass_kernel_spmd(nc, maps, core_ids=list(range(n)))
    outp = np.empty((B, S, D), np.float32)
    for cid in range(n):
        b, half = cid // 2, cid % 2
        outp[b, half * cfg.T:(half + 1) * cfg.T, :] = res.results[cid]["out"]
    return outp
```
